# Optimizing a Trainium2 kernel written in Bass

```python
import jax, jax.numpy as jnp
from jax import lax
import numpy as np

D_MODEL = 2048
BATCH = 4
SEQ = 8192
DEPTH = 1

CHUNK = 64
N_MEM = 256
EPS = 1e-6

GMLP_BLOCK = 128
GMLP_WIDTH = D_MODEL // 2
GMLP_GROUPS = 8
GMLP_GROUP_DIM = GMLP_WIDTH // GMLP_GROUPS
CONV_WIDTH = D_MODEL // 2
CONV_K = 3
XATTN_HEADS = 4
XATTN_HEAD_DIM = 256
XATTN_WIDTH = XATTN_HEADS * XATTN_HEAD_DIM
N_BRANCH = 3
BRANCH_WIDTH = D_MODEL // 2
COL_A = 2 * GMLP_WIDTH
COL_B = 3 * CONV_WIDTH
COL_C = XATTN_WIDTH
COL_G = N_BRANCH * D_MODEL
D_IN = COL_A + COL_B + COL_C + COL_G
PEER_HEADS = 8
PEER_KEYS = 128
PEER_EXPERTS = PEER_KEYS * PEER_KEYS
PEER_TOPK = 16
PEER_HALF = 128
PEER_TOKEN_BLOCK = 128

kernel_name = 'hybrid_streaming_gmlp_conv_xattn_peer'


def rmsnorm(x, g):
    xf = x.astype(jnp.float32)
    y = xf * lax.rsqrt(jnp.mean(jnp.square(xf), axis=-1, keepdims=True) + EPS)
    return (y * g).astype(x.dtype)


def layernorm(x, g, b):
    xf = x.astype(jnp.float32)
    mu = jnp.mean(xf, axis=-1, keepdims=True)
    var = jnp.mean(jnp.square(xf - mu), axis=-1, keepdims=True)
    return ((xf - mu) * lax.rsqrt(var + EPS) * g + b).astype(x.dtype)


def chunk_causal_block_mask():
    c = jnp.arange(GMLP_BLOCK) // CHUNK
    return c[None, :] <= c[:, None]


def spatial_gating(z, w_s, b_s, ln_g, ln_b):
    u, v = jnp.split(z, 2, axis=-1)
    v = layernorm(v, ln_g, ln_b)
    bsz, s, _ = v.shape
    nb = s // GMLP_BLOCK
    v = v.reshape(bsz, nb, GMLP_BLOCK, GMLP_GROUPS, GMLP_GROUP_DIM)
    w = jnp.where(chunk_causal_block_mask()[None], w_s, 0.0).astype(v.dtype)
    sv = jnp.einsum('gij,bnjgc->bnigc', w, v) + b_s.T[:, :, None]
    return u * sv.reshape(bsz, s, GMLP_WIDTH)


def short_conv_mixer(z, conv_w):
    b_gate, c_gate, h = jnp.split(z, 3, axis=-1)
    y = lax.conv_general_dilated(
        c_gate * h, conv_w[:, None, :].astype(h.dtype),
        window_strides=(1,), padding=[(CONV_K - 1, 0)],
        dimension_numbers=('NWC', 'WIO', 'NWC'),
        feature_group_count=CONV_WIDTH)
    return b_gate * y


def memory_cross_attention(q, mem_n, w_kv):
    bsz, s, _ = q.shape
    m = mem_n.shape[1]
    k, v = jnp.split(mem_n @ w_kv, 2, axis=-1)
    q = q.reshape(bsz, s, XATTN_HEADS, XATTN_HEAD_DIM)
    k = k.reshape(bsz, m, XATTN_HEADS, XATTN_HEAD_DIM)
    v = v.reshape(bsz, m, XATTN_HEADS, XATTN_HEAD_DIM)
    sc = jnp.einsum('bshd,bmhd->bhsm', q, k).astype(jnp.float32) * (XATTN_HEAD_DIM ** -0.5)
    p = jax.nn.softmax(sc, axis=-1).astype(v.dtype)
    o = jnp.einsum('bhsm,bmhd->bshd', p, v)
    return o.reshape(bsz, s, XATTN_WIDTH)


def hybrid_mixer(h, mem_n, w_in, w_s, b_s, ln_g, ln_b, conv_w, w_kv, w_branch, w_out):
    bsz, s, _ = h.shape
    z = h @ w_in
    z_a, z_b, z_c, z_g = jnp.split(z, [COL_A, COL_A + COL_B, COL_A + COL_B + COL_C], axis=-1)
    gates = z_g.reshape(bsz, s, N_BRANCH, D_MODEL)
    branches = (spatial_gating(jax.nn.gelu(z_a), w_s, b_s, ln_g, ln_b),
                short_conv_mixer(z_b, conv_w),
                memory_cross_attention(z_c, mem_n, w_kv))
    merged = jnp.zeros((bsz, s, D_MODEL), h.dtype)
    for n in range(N_BRANCH):
        merged = merged + jax.nn.sigmoid(gates[:, :, n]) * (branches[n] @ w_branch[n])
    return merged @ w_out


def peer_retrieve(h, w_q, sub_keys):
    t = h.shape[0]
    q = (h @ w_q).reshape(t, PEER_HEADS, 2, PEER_HALF)
    sc = jnp.einsum('thpc,hpkc->thpk', q, sub_keys).astype(jnp.float32)
    top_s, top_i = lax.top_k(sc, PEER_TOPK)
    cand_s = (top_s[:, :, 0, :, None] + top_s[:, :, 1, None, :]).reshape(t, PEER_HEADS, PEER_TOPK * PEER_TOPK)
    cand_i = (top_i[:, :, 0, :, None] * PEER_KEYS + top_i[:, :, 1, None, :]).reshape(t, PEER_HEADS, PEER_TOPK * PEER_TOPK)
    best_s, pos = lax.top_k(cand_s, PEER_TOPK)
    idx = jnp.take_along_axis(cand_i, pos, axis=-1)
    g = jax.nn.softmax(best_s, axis=-1)
    return idx, g


def peer_experts(h, idx, g, w_down, w_up):
    t, d = h.shape
    nb = t // PEER_TOKEN_BLOCK

    def block(args):
        hb, ib, gb = args
        u = jnp.take(w_down, ib, axis=0)
        a = jax.nn.gelu(jnp.einsum('thkd,td->thk', u, hb))
        v = jnp.take(w_up, ib, axis=0)
        return jnp.einsum('thk,thkd->td', a * gb.astype(a.dtype), v)

    out = lax.map(block, (h.reshape(nb, PEER_TOKEN_BLOCK, d),
                          idx.reshape(nb, PEER_TOKEN_BLOCK, PEER_HEADS, PEER_TOPK),
                          g.reshape(nb, PEER_TOKEN_BLOCK, PEER_HEADS, PEER_TOPK)))
    return out.reshape(t, d)


def setup_inputs(seed: int = 0) -> dict:
    key = jax.random.key(seed)
    ks = jax.random.split(key, 20)
    f32 = jnp.float32
    nrm = lambda k, shape, scale: jax.random.normal(k, shape, f32) * scale
    gain = lambda k, shape: 1.0 + 0.02 * jax.random.normal(k, shape, f32)
    L = DEPTH
    return {
        'x': jax.random.normal(ks[0], (BATCH, SEQ, D_MODEL), f32),
        'mem': jax.random.normal(ks[1], (BATCH, N_MEM, D_MODEL), f32),
        'norm_mix_g': gain(ks[2], (L, D_MODEL)),
        'norm_mem_g': gain(ks[3], (L, D_MODEL)),
        'w_in': nrm(ks[4], (L, D_MODEL, D_IN), D_MODEL ** -0.5),
        'gmlp_w_s': nrm(ks[5], (L, GMLP_GROUPS, GMLP_BLOCK, GMLP_BLOCK), 0.5 * GMLP_BLOCK ** -0.5),
        'gmlp_b_s': 1.0 + 0.01 * jax.random.normal(ks[6], (L, GMLP_GROUPS, GMLP_BLOCK), f32),
        'gmlp_ln_g': gain(ks[7], (L, GMLP_WIDTH)),
        'gmlp_ln_b': nrm(ks[8], (L, GMLP_WIDTH), 0.01),
        'conv_w': nrm(ks[9], (L, CONV_K, CONV_WIDTH), CONV_K ** -0.5),
        'w_kv': nrm(ks[10], (L, D_MODEL, 2 * XATTN_WIDTH), D_MODEL ** -0.5),
        'w_branch': nrm(ks[11], (L, N_BRANCH, BRANCH_WIDTH, D_MODEL), BRANCH_WIDTH ** -0.5),
        'w_out': nrm(ks[12], (L, D_MODEL, D_MODEL), D_MODEL ** -0.5),
        'norm_ffn_g': gain(ks[13], (L, D_MODEL)),
        'peer_w_q': nrm(ks[14], (L, D_MODEL, PEER_HEADS * 2 * PEER_HALF), D_MODEL ** -0.5),
        'peer_keys': nrm(ks[15], (L, PEER_HEADS, 2, PEER_KEYS, PEER_HALF), PEER_HALF ** -0.5),
        'peer_w_down': nrm(ks[16], (L, PEER_EXPERTS, D_MODEL), D_MODEL ** -0.5),
        'peer_w_up': nrm(ks[17], (L, PEER_EXPERTS, D_MODEL), PEER_HEADS ** -0.5),
        'norm_final_g': gain(ks[18], (D_MODEL,)),
    }


def reference(x, mem, norm_mix_g, norm_mem_g, w_in, gmlp_w_s, gmlp_b_s, gmlp_ln_g, gmlp_ln_b,
              conv_w, w_kv, w_branch, w_out, norm_ffn_g, peer_w_q, peer_keys, peer_w_down,
              peer_w_up, norm_final_g):
    bsz, s, d = x.shape
    for l in range(DEPTH):
        h = rmsnorm(x, norm_mix_g[l])
        m = rmsnorm(mem, norm_mem_g[l])
        x = x + hybrid_mixer(h, m, w_in[l], gmlp_w_s[l], gmlp_b_s[l], gmlp_ln_g[l], gmlp_ln_b[l],
                             conv_w[l], w_kv[l], w_branch[l], w_out[l])
        h = rmsnorm(x, norm_ffn_g[l]).reshape(bsz * s, d)
        idx, g = peer_retrieve(h, peer_w_q[l], peer_keys[l])
        x = x + peer_experts(h, idx, g, peer_w_down[l], peer_w_up[l]).reshape(bsz, s, d)
    return rmsnorm(x, norm_final_g)
```

```python
import numpy as np
from contextlib import ExitStack
import concourse.bass as bass
import concourse.mybir as mybir
from concourse.bass_utils import run_bass_kernel_spmd

F32 = mybir.dt.float32
BF16 = mybir.dt.bfloat16
AF = mybir.ActivationFunctionType
ALU = mybir.AluOpType
DT_SIZE = {F32: 4, BF16: 2}

P = 128
D = 2048
DIN = 12288
T = 512
NB = 4
NEXP = 16384
NTOK_CORE = 4096
EPS = 1e-6
NEG = -1.0e30


class View:
    __slots__ = ("buf", "lo", "hi", "ap", "dtype", "dense")

    def __init__(self, buf, lo, hi, ap, dtype=None, dense=True):
        self.buf = buf
        self.lo = lo
        self.hi = hi
        self.ap = ap
        self.dtype = dtype
        self.dense = dense

    def sub(self, a, b):
        sz = DT_SIZE[self.dtype]
        return View(self.buf, self.lo + a * sz, self.lo + b * sz, self.ap[:, a:b], self.dtype, True)

    def span(self, a, b, ap):
        sz = DT_SIZE[self.dtype]
        return View(self.buf, self.lo + a * sz, self.lo + b * sz, ap, self.dtype, False)


class Buf:
    def __init__(self, name, handle=None, excl=False):
        self.name = name
        self.h = handle
        self.excl = excl
        self.last = []
        self.writers = []
        self.readers = {}


class Sched:
    def __init__(self, nc):
        self.nc = nc
        self.ops = []
        self.engobj = {"pe": nc.tensor, "act": nc.scalar, "dve": nc.vector,
                       "pool": nc.gpsimd, "sp": nc.sync}

    def _deps_for(self, reads, writes, opid, engkey):
        deps = set()
        ex = {}
        for v in reads:
            if v.buf.excl:
                ex.setdefault(id(v.buf), [v.buf, False])
        for v in writes:
            if v.buf.excl:
                ex.setdefault(id(v.buf), [v.buf, False])[1] = True
        for b, isw in ex.values():
            for (o, pw) in b.last:
                deps.add((o, "raw" if (pw and not isw) else "waw"))
            b.last = [(opid, isw)]
        reads = [v for v in reads if not v.buf.excl]
        writes = [v for v in writes if not v.buf.excl]
        for v in reads:
            for (lo, hi, o, _d) in v.buf.writers:
                if lo < v.hi and v.lo < hi:
                    deps.add((o, "raw"))
        for v in writes:
            b = v.buf
            for (lo, hi, o, _d) in b.writers:
                if lo < v.hi and v.lo < hi:
                    deps.add((o, "waw"))
            for (lo, hi, _k), o in b.readers.items():
                if lo < v.hi and v.lo < hi:
                    deps.add((o, "war"))
        for v in writes:
            b = v.buf
            if v.dense:
                b.writers = [w for w in b.writers if not (v.lo <= w[0] and w[1] <= v.hi)]
                b.readers = {k: o for k, o in b.readers.items() if not (v.lo <= k[0] and k[1] <= v.hi)}
            else:
                b.writers = [w for w in b.writers if not (w[0] == v.lo and w[1] == v.hi and not w[3])]
            b.writers.append((v.lo, v.hi, opid, v.dense))
        for v in reads:
            v.buf.readers[(v.lo, v.hi, engkey)] = opid
        return deps

    def op(self, eng, fn, reads=(), writes=(), dma_sem=None):
        opid = len(self.ops)
        engkey = eng if dma_sem is None else ("dma", dma_sem)
        deps = self._deps_for(reads, writes, opid, engkey)
        self.ops.append(dict(eng=eng, fn=fn, deps=deps, dma_sem=dma_sem, signal=False, barrier=False))
        return opid

    def dma_barrier(self, eng="sp"):
        self.ops.append(dict(eng=eng, fn=None, deps=set(), dma_sem=None, signal=False, barrier=True))

    def emit(self, stack):
        nc = self.nc
        ops = self.ops
        for o in ops:
            need = set()
            for (p, kind) in o["deps"]:
                po = ops[p]
                if po["dma_sem"] is None and po["eng"] == o["eng"] and o["dma_sem"] is None:
                    if o["eng"] == "pe":
                        continue
                need.add(p)
            latest = {}
            keep = set()
            for p in need:
                po = ops[p]
                if po["dma_sem"] is not None:
                    keep.add(p)
                elif po["eng"] not in latest or p > latest[po["eng"]]:
                    latest[po["eng"]] = p
            keep.update(latest.values())
            o["need"] = keep
            for p in keep:
                ops[p]["signal"] = True
        sems = {}
        for e in ("pe", "act", "dve", "pool"):
            sems[e] = stack.enter_context(nc.semaphore("s_" + e))
        for o in ops:
            if o["dma_sem"] is not None:
                k = ("dma", o["dma_sem"])
                if k not in sems:
                    sems[k] = stack.enter_context(nc.semaphore("d_" + str(o["dma_sem"])))
        count = {k: 0 for k in sems}
        waited = {e: {} for e in self.engobj}
        nwait = 0
        for o in ops:
            e = o["eng"]
            eo = self.engobj[e]
            if o["barrier"]:
                for sk, val in count.items():
                    if isinstance(sk, tuple) and val > 0 and waited[e].get(sk, 0) < val:
                        eo.wait_ge(sems[sk], val)
                        waited[e][sk] = val
                        nwait += 1
                continue
            tg = {}
            for p in o["need"]:
                po = ops[p]
                sk, val = po["sig"]
                if po["dma_sem"] is not None:
                    val = count[sk]
                if tg.get(sk, 0) < val:
                    tg[sk] = val
            for sk, val in tg.items():
                if waited[e].get(sk, 0) >= val:
                    continue
                eo.wait_ge(sems[sk], val)
                waited[e][sk] = val
                nwait += 1
            ins = o["fn"](eo)
            if o["dma_sem"] is not None:
                sk = ("dma", o["dma_sem"])
                count[sk] += 16
                ins.then_inc(sems[sk], 16)
                o["sig"] = (sk, count[sk])
            elif o["signal"]:
                count[e] += 1
                ins.then_inc(sems[e], 1)
                o["sig"] = (e, count[e])
            else:
                o["sig"] = None
        self.nwait = nwait
        self.nsem = len(sems)
        self.counts = count


class _Stop(Exception):
    pass


def build(ntiles=8, debug=False, stop=None):
    nc = bass.Bass("TRN2", target_bir_lowering=False)
    ntok = ntiles * T

    def din(name, shape):
        return nc.dram_tensor(name, list(shape), F32, kind="ExternalInput").ap()

    x_d = din("x", [ntok, D])
    xh_d = din("xh", [2, D])
    mem_d = din("mem", [256, D])
    g1_d = din("norm_mix_g", [D])
    gm_d = din("norm_mem_g", [D])
    win_d = din("w_in", [D, DIN])
    ws_d = din("gmlp_w_s", [8, 128, 128])
    bs_d = din("gmlp_b_s", [8 * 128])
    lng_d = din("gmlp_ln_g", [1024])
    lnb_d = din("gmlp_ln_b", [1024])
    cw_d = din("conv_w", [3, 1024])
    wkv_d = din("w_kv", [D, 2048])
    wbr_d = din("w_branch", [3, 1024, D])
    wout_d = din("w_out", [D, D])
    g2_d = din("norm_ffn_g", [D])
    wq_d = din("peer_w_q", [D, 2048])
    keys_d = din("peer_keys", [16, 128, 128])
    wdn_d = din("peer_w_down", [NEXP, D])
    wup_d = din("peer_w_up", [NEXP, D])
    gf_d = din("norm_final_g", [D])
    ident_d = din("ident", [128, 128])
    y_d = nc.dram_tensor("y", [ntok, D], F32, kind="ExternalOutput").ap()

    def dscr(name, shape):
        return nc.dram_tensor(name, list(shape), BF16, kind="Internal").ap()

    s_win = dscr("s_win", [24, 128, 16, 512])
    s_wbr = dscr("s_wbr", [12, 128, 8, 512])
    s_wout = dscr("s_wout", [4, 128, 16, 512])
    s_wq = dscr("s_wq", [4, 128, 16, 512])
    s_wdT = dscr("s_wdT", [64, 128, 16, 256])
    s_wup = dscr("s_wup", [128, 128, 4, 512])
    s_small = dscr("s_small", [128, 7168])

    dbg = {}

    st = ExitStack()
    with st:
        S = Sched(nc)
        ARENA_BYTES = 207 * 1024
        A = st.enter_context(nc.sbuf_tensor("arena", [128, ARENA_BYTES // 4], F32))
        abuf = Buf("arena", A)
        off = [0]

        def alloc(n, dt):
            nb = (n * DT_SIZE[dt] + 3) // 4 * 4
            lo = off[0]
            off[0] += nb
            assert off[0] <= ARENA_BYTES, ("arena overflow", off[0])
            ap = A[:, lo // 4:(lo + nb) // 4]
            if dt != F32:
                ap = ap.bitcast(dt)
            return View(abuf, lo, lo + nb, ap, dt)

        banks = []
        for i in range(8):
            t_ = st.enter_context(nc.psum_tensor("bank%d" % i, [128, 512], F32))
            banks.append(Buf("bank%d" % i, t_, excl=True))
        bctr = [0]

        def bank(dt=F32):
            i = bctr[0] % 8
            bctr[0] += 1
            ap = banks[i].h[:, :]
            if dt != F32:
                ap = ap.bitcast(dt)
            return View(banks[i], 0, 2048, ap, dt)

        rd_only = Buf("dram_ro")
        ybuf = Buf("y")
        scr_w = Buf("scratch_w")
        wctr = [0]

        def DR(ap):
            return View(rd_only, 0, 1, ap, None)

        def DW(ap):
            wctr[0] += 1
            return View(scr_w, wctr[0], wctr[0] + 1, ap, None)

        def dma(outv, inv, sem, eng="sp", **kw):
            S.op(eng, lambda e, o=outv.ap, i=inv.ap, kw=kw: e.dma_start(out=o, in_=i, **kw),
                 reads=[inv], writes=[outv], dma_sem=sem)

        def mm(outv, out_ap, lv, l_ap, rv, r_ap, start, stop):
            S.op("pe", lambda e, o=out_ap, l=l_ap, r=r_ap, a=start, b=stop:
                 e.matmul(o, lhsT=l, rhs=r, start=a, stop=b), reads=[lv, rv], writes=[outv])

        def tr(outv, out_ap, inv, in_ap, idv):
            S.op("pe", lambda e, o=out_ap, i=in_ap, d=idv.ap: e.transpose(out=o, in_=i, identity=d),
                 reads=[inv, idv], writes=[outv])

        def act(outv, out_ap, inv, in_ap, func, extra_r=(), extra_w=(), **kw):
            S.op("act", lambda e, o=out_ap, i=in_ap, f=func, kw=kw: e.activation(out=o, in_=i, func=f, **kw),
                 reads=[inv] + list(extra_r), writes=[outv] + list(extra_w))

        def tcopy(eng, outv, out_ap, inv, in_ap):
            if eng == "act":
                S.op("act", lambda e, o=out_ap, i=in_ap: e.copy(out=o, in_=i), reads=[inv], writes=[outv])
            else:
                S.op(eng, lambda e, o=out_ap, i=in_ap: e.tensor_copy(out=o, in_=i), reads=[inv], writes=[outv])

        def tt(eng, outv, out_ap, av, a_ap, bv, b_ap, op):
            S.op(eng, lambda e, o=out_ap, a=a_ap, b=b_ap, op=op: e.tensor_tensor(out=o, in0=a, in1=b, op=op),
                 reads=[av, bv], writes=[outv])

        def ts(eng, outv, out_ap, inv, in_ap, s1, op0, s2=None, op1=None, extra_r=()):
            def f(e, o=out_ap, i=in_ap, s1=s1, s2=s2, op0=op0, op1=op1):
                if op1 is None:
                    return e.tensor_scalar(out=o, in0=i, scalar1=s1, scalar2=None, op0=op0)
                return e.tensor_scalar(out=o, in0=i, scalar1=s1, scalar2=s2, op0=op0, op1=op1)
            S.op(eng, f, reads=[inv] + list(extra_r), writes=[outv])

        def stt(outv, out_ap, av, a_ap, scalar, bv, b_ap, op0, op1, extra_r=()):
            S.op("dve", lambda e, o=out_ap, a=a_ap, s=scalar, b=b_ap, op0=op0, op1=op1:
                 e.scalar_tensor_tensor(out=o, in0=a, scalar=s, in1=b, op0=op0, op1=op1),
                 reads=[av, bv] + list(extra_r), writes=[outv])

        def memset(eng, v, ap, val):
            S.op(eng, lambda e, a=ap, c=val: e.memset(a, c), writes=[v])

        rr = [0]

        def rr_eng(choices):
            rr[0] += 1
            return choices[rr[0] % len(choices)]

        xs = alloc(NB * D, F32)
        hT = alloc(16 * T, BF16)
        identf = alloc(128, F32)
        identb = alloc(128, BF16)
        onesb = alloc(128, BF16)
        convw = alloc(8 * 3, F32)
        g1f = alloc(16, F32)
        g2f = alloc(16, F32)
        gmf = alloc(16, F32)
        gT = alloc(8 * 516, BF16)
        stat = alloc(64, F32)
        PERSIST = off[0]

        def hT_k(k):
            return hT.sub(k * T, (k + 1) * T)

        dctr = [0]

        def dump(name, srcv, shape, dt=F32):
            o = nc.dram_tensor("dbg_" + name, list(shape), dt, kind="ExternalOutput").ap()
            dctr[0] += 1
            dma(View(ybuf, 10 ** 9 + dctr[0], 10 ** 9 + dctr[0] + 1, o), srcv, "dbg%d" % dctr[0])

        def checkpoint(name):
            if stop == name:
                raise _Stop()

        try:
            dma(identf, DR(ident_d), "c_id")
            tcopy("dve", identb, identb.ap, identf, identf.ap)
            memset("dve", onesb, onesb.ap, 1.0)
            for k_ in range(3):
                dma(convw.sub(k_ * 8, k_ * 8 + 8), DR(cw_d[k_].rearrange("(c p) -> p c", p=128)), "c_cw", allow_slow_non_contiguous=True)
            dma(g1f, DR(g1_d.rearrange("(k p) -> p k", p=128)), "c_g1", allow_slow_non_contiguous=True)
            dma(g2f, DR(g2_d.rearrange("(k p) -> p k", p=128)), "c_g2", allow_slow_non_contiguous=True)
            dma(gmf, DR(gm_d.rearrange("(k p) -> p k", p=128)), "c_gm", allow_slow_non_contiguous=True)

            def norm_transpose(src_v, nblk, dstT, dst_cols, tmp_hb, tmp_junk):
                for b in range(nblk):
                    xb = src_v.sub(b * D, (b + 1) * D)
                    ss = stat.sub(b, b + 1)
                    rs = stat.sub(8 + b, 9 + b)
                    hb = tmp_hb[b % len(tmp_hb)]
                    tmp_junk = hb
                    memset("pool", ss, ss.ap, 0.0)
                    act(tmp_junk, tmp_junk.ap, xb, xb.ap, AF.Square, extra_r=[ss], extra_w=[ss], accum_out=ss.ap)
                    ts("dve", rs, rs.ap, ss, ss.ap, 1.0 / D, ALU.mult, EPS, ALU.add)
                    act(rs, rs.ap, rs, rs.ap, AF.Sqrt)
                    S.op("dve", lambda e, o=rs.ap: e.reciprocal(out=o, in_=o), reads=[rs], writes=[rs])
                    act(hb, hb.ap, xb, xb.ap, AF.Copy, extra_r=[rs], scale=rs.ap)
                    for half in range(2):
                        bk = bank(BF16)
                        for kk in range(8):
                            k = half * 8 + kk
                            tr(bk, bk.ap[:, kk * 128:(kk + 1) * 128], hb, hb.ap[:, k * 128:(k + 1) * 128], identb)
                        a0 = half * 8 * dst_cols + b * 128
                        a1 = (half * 8 + 7) * dst_cols + (b + 1) * 128
                        dap = dstT.ap.rearrange("p (k s) -> p k s", s=dst_cols)[:, half * 8:half * 8 + 8, b * 128:(b + 1) * 128]
                        dv = dstT.span(a0, a1, dap)
                        tcopy(rr_eng(["act", "dve"]), dv, dap, bk, bk.ap.rearrange("p (k s) -> p k s", s=128))

            PRO = off[0]
            NSTG = 4
            stg = [alloc(4096, F32) for _ in range(NSTG)]
            stgb = [alloc(4096, BF16) for _ in range(NSTG)]
            pctr = [0]
            small = alloc(7168, BF16)
            kT = small.sub(0, 2048)
            vtok = small.sub(2048, 4096)
            wsT = small.sub(4096, 5120)
            keysT = small.sub(5120, 7168)

            pieces = []

            def run_pieces():
                n = len(pieces)
                base = pctr[0]
                for j in range(min(NSTG - 1, n)):
                    pieces[j][0]((base + j) % NSTG)
                for i_ in range(n):
                    j = i_ + NSTG - 1
                    if j < n:
                        pieces[j][0]((base + j) % NSTG)
                    pieces[i_][1]((base + i_) % NSTG)
                pctr[0] += n
                del pieces[:]

            def cast_rows(w_ap, nrows, ncols, gain, store_fn):
                cp = min(ncols, 4096)
                for k in range(nrows // 128):
                    for c0 in range(0, ncols, cp):
                        def ld(i, k=k, c0=c0):
                            sv = stg[i].sub(0, cp)
                            dma(sv, DR(w_ap[k * 128:(k + 1) * 128, c0:c0 + cp]), "stg%d" % i)

                        def wk(i, k=k, c0=c0):
                            sv = stg[i].sub(0, cp)
                            bv = stgb[i].sub(0, cp)
                            eng = rr_eng(["dve", "act", "act"])
                            if gain is None:
                                tcopy(eng, bv, bv.ap, sv, sv.ap)
                            elif eng == "act":
                                act(bv, bv.ap, sv, sv.ap, AF.Copy, extra_r=[gain], scale=gain.ap[:, k:k + 1])
                            else:
                                ts(eng, bv, bv.ap, sv, sv.ap, gain.ap[:, k:k + 1], ALU.mult, extra_r=[gain])
                            store_fn(k, c0, cp, bv, "stgb%d" % i)
                        pieces.append((ld, wk))

            def st_win(k, c0, cp, bv, sem):
                g0 = c0 // 512
                ng = cp // 512
                dma(DW(s_win[g0:g0 + ng, :, k, :].rearrange("g p c -> p g c")),
                    View(bv.buf, bv.lo, bv.hi, bv.ap.rearrange("p (g c) -> p g c", c=512)), sem)
            cast_rows(win_d, D, DIN, g1f, st_win)

            for n in range(3):
                def st_wbr(k, c0, cp, bv, sem, n=n):
                    dma(DW(s_wbr[n * 4:(n + 1) * 4, :, k, :].rearrange("g p c -> p g c")),
                        View(bv.buf, bv.lo, bv.hi, bv.ap.rearrange("p (g c) -> p g c", c=512)), sem)
                cast_rows(wbr_d[n], 1024, D, None, st_wbr)

            def st_wout(k, c0, cp, bv, sem):
                dma(DW(s_wout[:, :, k, :].rearrange("g p c -> p g c")),
                    View(bv.buf, bv.lo, bv.hi, bv.ap.rearrange("p (g c) -> p g c", c=512)), sem)
            cast_rows(wout_d, D, D, None, st_wout)

            def st_wq(k, c0, cp, bv, sem):
                dma(DW(s_wq[:, :, k, :].rearrange("g p c -> p g c")),
                    View(bv.buf, bv.lo, bv.hi, bv.ap.rearrange("p (g c) -> p g c", c=512)), sem)
            cast_rows(wq_d, D, 2048, g2f, st_wq)

            def st_wup(k, c0, cp, bv, sem):
                r, cc = k // 4, k % 4
                dma(DW(s_wup[r * 4:(r + 1) * 4, :, cc, :].rearrange("g p c -> p g c")),
                    View(bv.buf, bv.lo, bv.hi, bv.ap.rearrange("p (g c) -> p g c", c=512)), sem)
            cast_rows(wup_d, NEXP, D, None, st_wup)
            run_pieces()

            PRO2 = off[0]
            stgT = [alloc(16 * 512, BF16) for _ in range(2)]
            for eb in range(NEXP // 128):
                def ld(i, eb=eb):
                    sv = stg[i].sub(0, D)
                    dma(sv, DR(wdn_d[eb * 128:(eb + 1) * 128, :]), "stg%d" % i)

                def wk(i, eb=eb):
                    r, cc = eb // 4, eb % 4
                    sv = stg[i].sub(0, D)
                    tv = stgT[r % 2]
                    for q in range(4):
                        bk = bank()
                        for kk in range(4):
                            k = q * 4 + kk
                            tr(bk, bk.ap[:, kk * 128:(kk + 1) * 128], sv, sv.ap[:, k * 128:(k + 1) * 128], identf)
                        eng = rr_eng(["dve", "act"])
                        for kk in range(4):
                            k = q * 4 + kk
                            a0 = k * 512 + cc * 128
                            dv = tv.sub(a0, a0 + 128)
                            src = View(bk.buf, kk * 512, kk * 512 + 512, bk.ap[:, kk * 128:(kk + 1) * 128], F32)
                            if eng == "act":
                                act(dv, dv.ap, src, src.ap, AF.Copy, extra_r=[g2f], scale=g2f.ap[:, k:k + 1])
                            else:
                                ts("dve", dv, dv.ap, src, src.ap, g2f.ap[:, k:k + 1], ALU.mult, extra_r=[g2f])
                    if cc == 3:
                        for half in range(2):
                            sap = tv.ap.rearrange("p (k e) -> p k e", e=512)[:, :, half * 256:(half + 1) * 256]
                            dma(DW(s_wdT[r * 2 + half]), tv.span(0, 16 * 512, sap), "stgT%d" % (r % 2))
                pieces.append((ld, wk))
            run_pieces()

            ktmp = stg[0].sub(0, 128)
            for hp in range(16):
                kv_ = stg[hp % 2].sub(0, 128)
                dma(kv_, DR(keys_d[hp]), "stg%d" % (hp % 2))
                bk = bank()
                tr(bk, bk.ap[:, 0:128], kv_, kv_.ap, identf)
                dv = keysT.sub(hp * 128, (hp + 1) * 128)
                tcopy("dve", dv, dv.ap, View(bk.buf, 0, 512, bk.ap[:, 0:128], F32), bk.ap[:, 0:128])
            for g in range(8):
                wv_ = stg[g % 2].sub(0, 128)
                dma(wv_, DR(ws_d[g]), "stg%d" % (g % 2))
                bk = bank()
                tr(bk, bk.ap[:, 0:128], wv_, wv_.ap, identf)
                dv = wsT.sub(g * 128, (g + 1) * 128)
                tcopy("dve", dv, dv.ap, View(bk.buf, 0, 512, bk.ap[:, 0:128], F32), bk.ap[:, 0:128])
                memset("dve", dv, dv.ap[64:128, 0:64], 0.0)

            off[0] = PRO2
            memx = alloc(2 * D, F32)
            memT = alloc(16 * 256, BF16)
            hb_m = [alloc(D, BF16)]
            junk_m = None
            wkb = [alloc(1024, BF16) for _ in range(2)]
            for b in range(2):
                dma(memx.sub(b * D, (b + 1) * D), DR(mem_d[b * 128:(b + 1) * 128, :]), "memx")
            norm_transpose(memx, 2, memT, 256, hb_m, junk_m)
            for part in range(2):
                accs = [bank() for _ in range(8 if part == 0 else 4)]
                for k in range(16):
                    i = pctr[0] % NSTG
                    pctr[0] += 1
                    sv = stg[i].sub(0, 1024)
                    dma(sv, DR(wkv_d[k * 128:(k + 1) * 128, part * 1024:(part + 1) * 1024]), "stg%d" % i)
                    wb = wkb[k % 2]
                    ts("dve", wb, wb.ap, sv, sv.ap, gmf.ap[:, k:k + 1], ALU.mult, extra_r=[gmf])
                    mk = memT.sub(k * 256, (k + 1) * 256)
                    if part == 0:
                        for c in range(8):
                            mm(accs[c], accs[c].ap[:, 0:256], wb, wb.ap[:, c * 128:(c + 1) * 128], mk, mk.ap, k == 0, k == 15)
                    else:
                        for mc in range(2):
                            for cg in range(2):
                                a = accs[mc * 2 + cg]
                                mm(a, a.ap, mk, mk.ap[:, mc * 128:(mc + 1) * 128], wb, wb.ap[:, cg * 512:(cg + 1) * 512], k == 0, k == 15)
                if part == 0:
                    for c in range(8):
                        dv = kT.sub(c * 256, (c + 1) * 256)
                        tcopy(rr_eng(["act", "dve"]), dv, dv.ap, accs[c], accs[c].ap[:, 0:256])
                else:
                    for mc in range(2):
                        for cg in range(2):
                            dv = vtok.sub(mc * 1024 + cg * 512, mc * 1024 + (cg + 1) * 512)
                            tcopy(rr_eng(["act", "dve"]), dv, dv.ap, accs[mc * 2 + cg], accs[mc * 2 + cg].ap)

            dma(DW(s_small), small, "small_st")
            S.dma_barrier("sp")
            if stop == "pro":
                dump("kT", kT, [128, 8 * 256], BF16)
                dump("vtok", vtok, [128, 2048], BF16)
                dump("keysT", keysT, [128, 2048], BF16)
                dump("wsT", wsT, [128, 1024], BF16)
                dump("memT", memT, [128, 16 * 256], BF16)
                dump("s_win0", DR(s_win[0]), [128, 16, 512], BF16)
                dump("s_win23", DR(s_win[23]), [128, 16, 512], BF16)
                dump("s_wbr5", DR(s_wbr[5]), [128, 8, 512], BF16)
                dump("s_wout1", DR(s_wout[1]), [128, 16, 512], BF16)
                dump("s_wq2", DR(s_wq[2]), [128, 16, 512], BF16)
                dump("s_wdT3", DR(s_wdT[3]), [128, 16, 256], BF16)
                dump("s_wup5", DR(s_wup[5]), [128, 4, 512], BF16)
            checkpoint("pro")
            off[0] = PRO

            MIX = off[0]
            win_s = [alloc(16 * 512, BF16) for _ in range(2)]
            wctr2 = [0]
            yT = alloc(24 * T, BF16)
            sgt = alloc(8 * T, BF16)
            wbr_s = [alloc(8 * 512, BF16) for _ in range(2)]
            hbs = [alloc(D, BF16) for _ in range(2)]
            junk = None
            msmall = alloc(5120, BF16)
            kT = msmall.sub(0, 2048)
            vtok = msmall.sub(2048, 4096)
            wsT = msmall.sub(4096, 5120)
            bsb = alloc(8 * 128, F32)
            SCR = off[0]
            uT = alloc(8 * T, BF16)
            vtm = alloc(NB * 1024, F32)
            vln = alloc(NB * 1024, BF16)
            lng = alloc(1024, F32)
            lnb = alloc(1024, F32)
            bnst = alloc(16, F32)
            tmpA = alloc(T, F32)
            SCR_END = off[0]
            off[0] = SCR
            cT = alloc(8 * T, BF16)
            tcv = alloc(2 * T, F32)
            off[0] = SCR
            qT = alloc(8 * T, BF16)
            pT = alloc(2 * 2 * T, BF16)
            rD = alloc(2 * T, F32)
            off[0] = SCR
            mT = alloc(16 * T, BF16)
            mtmp = alloc(2 * T, F32)
            assert off[0] <= SCR_END
            off[0] = SCR_END
            MIX_END = off[0]

            off[0] = MIX
            sc = alloc(NB * 16 * 128, F32)
            c24 = alloc(8 * 24, F32)
            pst = alloc(NB * 8 * 4, F32)
            ecb = alloc(NB * 8, F32)
            dgm = alloc(NB * 8 * 128, BF16)
            wd_s = [alloc(16 * 256, BF16) for _ in range(3)]
            wu_s = [alloc(4 * 512, BF16) for _ in range(4)]
            GA = off[0]
            GDN = 6
            gd2 = [alloc(4 * T, BF16) for _ in range(GDN)]
            ATB = off[0]
            AT2 = [alloc(4 * T, BF16) for _ in range(2)]
            GA_END = off[0]
            off[0] = ATB
            top = alloc(NB * 16 * 16, F32)
            candb = alloc(2 * 256, F32)
            assert off[0] <= GA_END
            off[0] = GA_END
            GRID = off[0]
            NCH = 2
            Sg2 = [alloc(4096, BF16) for _ in range(NCH)]
            Wg2 = [alloc(4096, BF16) for _ in range(NCH)]
            GRID_END = off[0]
            off[0] = GRID
            q2T = alloc(16 * T, BF16)
            keysT = alloc(16 * 128, BF16)
            cand8 = alloc(8 * 256, F32)
            penb = alloc(16 * 128, BF16)
            assert off[0] <= GRID_END
            PEER_END = GRID_END
            total_bytes = max(MIX_END, PEER_END)
            assert total_bytes <= ARENA_BYTES, total_bytes

            def bankx(i, dt=F32):
                ap = banks[i].h[:, :]
                if dt != F32:
                    ap = ap.bitcast(dt)
                return View(banks[i], 0, 2048, ap, dt)

            def load_win(g):
                w = win_s[wctr2[0] % 2]
                sem = "win%d" % (wctr2[0] % 2)
                wctr2[0] += 1
                dma(w, DR(s_win[g]), sem)
                return w

            def proj_fm(w, cc, evac):
                bk = bank()
                for k in range(16):
                    wk = w.sub(k * 512 + cc * 128, k * 512 + (cc + 1) * 128)
                    mm(bk, bk.ap, wk, wk.ap, hT_k(k), hT_k(k).ap, k == 0, k == 15)
                evac(bk)

            def conv_gate_cols(ncols, dst_col0):
                for gi in (6, 7):
                    w = load_win(gi)
                    for cc in range(4):
                        ch = (gi - 6) * 4 + cc
                        bk = bank()
                        for k in range(16):
                            wk = w.sub(k * 512 + cc * 128, k * 512 + (cc + 1) * 128)
                            hk = hT.sub(k * T, k * T + ncols)
                            mm(bk, bk.ap[:, 0:ncols], wk, wk.ap, hk, hk.ap, k == 0, k == 15)
                        dv = cT.sub(ch * T, ch * T + ncols)
                        tcopy("act", dv, dv.ap, bk, bk.ap[:, 0:ncols])
                for gi in (8, 9):
                    w = load_win(gi)
                    for cc in range(4):
                        ch = (gi - 8) * 4 + cc
                        bk = bank()
                        for k in range(16):
                            wk = w.sub(k * 512 + cc * 128, k * 512 + (cc + 1) * 128)
                            hk = hT.sub(k * T, k * T + ncols)
                            mm(bk, bk.ap[:, 0:ncols], wk, wk.ap, hk, hk.ap, k == 0, k == 15)
                        dv = gT.sub(ch * 516 + dst_col0, ch * 516 + dst_col0 + ncols)
                        cv = cT.sub(ch * T, ch * T + ncols)
                        tt("dve", dv, dv.ap, bk, bk.ap[:, 0:ncols], cv, cv.ap, ALU.mult)

            xh_v = xs.sub(0, D)
            memset("dve", xh_v, xh_v.ap, 0.0)
            dma(View(abuf, xh_v.lo, xh_v.hi, xh_v.ap[0:2, :], F32), DR(xh_d), "xs0")
            norm_transpose(xs, 1, hT, T, hbs, junk)
            conv_gate_cols(2, 514)

            for t in range(ntiles):
                r0 = t * T
                for b in range(NB):
                    dma(xs.sub(b * D, (b + 1) * D), DR(x_d[r0 + b * 128:r0 + (b + 1) * 128, :]), "xs%d" % b)
                dma(lng, DR(lng_d.partition_broadcast(128)), "lng")
                dma(lnb, DR(lnb_d.partition_broadcast(128)), "lnb")
                dma(msmall, DR(s_small[:, 0:5120]), "msmall")
                dma(bsb, DR(bs_d.partition_broadcast(128)), "c_bs")
                norm_transpose(xs, NB, hT, T, hbs, junk)

                for gi in (0, 1):
                    w = load_win(gi)
                    for cc in range(4):
                        ch = gi * 4 + cc
                        dv = uT.sub(ch * T, (ch + 1) * T)
                        proj_fm(w, cc, lambda bk, dv=dv: act(dv, dv.ap, bk, bk.ap, AF.Gelu_apprx_tanh))
                for gi in (2, 3):
                    w = load_win(gi)
                    for b in range(NB):
                        bk = bank()
                        for k in range(16):
                            hk = hT.sub(k * T + b * 128, k * T + (b + 1) * 128)
                            wk = w.sub(k * 512, (k + 1) * 512)
                            mm(bk, bk.ap, hk, hk.ap, wk, wk.ap, k == 0, k == 15)
                        dv = vtm.sub(b * 1024 + (gi - 2) * 512, b * 1024 + (gi - 1) * 512)
                        act(dv, dv.ap, bk, bk.ap, AF.Gelu_apprx_tanh)
                for b in range(NB):
                    vb = vtm.sub(b * 1024, (b + 1) * 1024)
                    S.op("dve", lambda e, o=bnst.ap[:, 0:6], i=vb.ap[:, 0:512]: e.bn_stats(out=o, in_=i), reads=[vb], writes=[bnst.sub(0, 6)])
                    S.op("dve", lambda e, o=bnst.ap[:, 6:12], i=vb.ap[:, 512:1024]: e.bn_stats(out=o, in_=i), reads=[vb], writes=[bnst.sub(6, 12)])
                    mv = bnst.sub(12, 14)
                    S.op("dve", lambda e, o=mv.ap, i=bnst.ap[:, 0:12]: e.bn_aggr(out=o, in_=i), reads=[bnst.sub(0, 12)], writes=[mv])
                    rs = bnst.sub(14, 15)
                    ts("dve", rs, rs.ap, mv, mv.ap[:, 1:2], EPS, ALU.add)
                    act(rs, rs.ap, rs, rs.ap, AF.Sqrt)
                    S.op("dve", lambda e, o=rs.ap: e.reciprocal(out=o, in_=o), reads=[rs], writes=[rs])
                    stt(vb, vb.ap, vb, vb.ap, mv.ap[:, 0:1], lng, lng.ap, ALU.subtract, ALU.mult, extra_r=[mv])
                    vl = vln.sub(b * 1024, (b + 1) * 1024)
                    stt(vl, vl.ap, vb, vb.ap, rs.ap, lnb, lnb.ap, ALU.mult, ALU.add, extra_r=[rs])
                for g in range(8):
                    bk = bank()
                    for b in range(NB):
                        vl = vln.sub(b * 1024 + g * 128, b * 1024 + (g + 1) * 128)
                        wg_ = wsT.sub(g * 128, (g + 1) * 128)
                        mm(bk, bk.ap[:, b * 128:(b + 1) * 128], vl, vl.ap, wg_, wg_.ap, True, True)
                    tmpv = tmpA
                    bsg = bsb.sub(g * 128, (g + 1) * 128)
                    tt("dve", tmpv, tmpv.ap.rearrange("p (b i) -> p b i", i=128), bk, bk.ap.rearrange("p (b i) -> p b i", i=128),
                       bsg, bsg.ap.unsqueeze(1).to_broadcast([128, NB, 128]), ALU.add)
                    dv = yT.sub(g * T, (g + 1) * T)
                    uv = uT.sub(g * T, (g + 1) * T)
                    tt("pool", dv, dv.ap, tmpv, tmpv.ap, uv, uv.ap, ALU.mult)

                gsrc = gT.ap.rearrange("p (c s) -> p c s", s=516)
                S.op("dve", lambda e, o=gsrc[:, :, 0:2], i=gsrc[:, :, 514:516]: e.tensor_copy(out=o, in_=i),
                     reads=[gT.span(0, 8 * 516, None)], writes=[gT.span(0, 8 * 516, None)])
                conv_gate_cols(T, 2)
                gsrc2 = gT.ap.rearrange("p (c s) -> p c s", s=516)
                S.op("pool", lambda e, o=gsrc2[:, :, 514:516], i=gsrc2[:, :, 512:514]: e.tensor_copy(out=o, in_=i),
                     reads=[gT.span(0, 8 * 516, None)], writes=[gT.span(0, 8 * 516, None)])
                for gi in (4, 5):
                    w = load_win(gi)
                    for cc in range(4):
                        ch = (gi - 4) * 4 + cc
                        gch = gT.sub(ch * 516, (ch + 1) * 516)
                        tv_ = tcv.sub((ch % 2) * T, (ch % 2 + 1) * T)
                        cwv = convw
                        ts("dve", tv_, tv_.ap, gch, gch.ap[:, 2:514], cwv.ap[:, 16 + ch:17 + ch], ALU.mult, extra_r=[cwv])
                        stt(tv_, tv_.ap, gch, gch.ap[:, 1:513], cwv.ap[:, 8 + ch:9 + ch], tv_, tv_.ap, ALU.mult, ALU.add, extra_r=[cwv])
                        stt(tv_, tv_.ap, gch, gch.ap[:, 0:512], cwv.ap[:, ch:ch + 1], tv_, tv_.ap, ALU.mult, ALU.add, extra_r=[cwv])
                        dv = yT.sub((8 + ch) * T, (9 + ch) * T)
                        proj_fm(w, cc, lambda bk, dv=dv, tv_=tv_: tt("dve", dv, dv.ap, bk, bk.ap, tv_, tv_.ap, ALU.mult))

                for gi in (10, 11):
                    w = load_win(gi)
                    for cc in range(4):
                        ch = (gi - 10) * 4 + cc
                        dv = qT.sub(ch * T, (ch + 1) * T)
                        proj_fm(w, cc, lambda bk, dv=dv: act(dv, dv.ap, bk, bk.ap, AF.Copy, scale=0.0625))
                for hd in range(4):
                    pv = pT.sub((hd % 2) * 2 * T, (hd % 2 + 1) * 2 * T)
                    for mc in range(2):
                        bk = bank()
                        for dc in range(2):
                            kk = kT.sub((2 * hd + dc) * 256 + mc * 128, (2 * hd + dc) * 256 + (mc + 1) * 128)
                            qq = qT.sub((2 * hd + dc) * T, (2 * hd + dc + 1) * T)
                            mm(bk, bk.ap, kk, kk.ap, qq, qq.ap, dc == 0, dc == 1)
                        pm = pv.sub(mc * T, (mc + 1) * T)
                        act(pm, pm.ap, bk, bk.ap, AF.Exp)
                    bD = bank()
                    for mc in range(2):
                        pm = pv.sub(mc * T, (mc + 1) * T)
                        mm(bD, bD.ap, onesb, onesb.ap, pm, pm.ap, mc == 0, mc == 1)
                    rv = rD.sub((hd % 2) * T, (hd % 2 + 1) * T)
                    S.op("dve", lambda e, o=rv.ap, i=bD.ap: e.reciprocal(out=o, in_=i), reads=[bD], writes=[rv])
                    for dc in range(2):
                        bO = bank()
                        for mc in range(2):
                            vv = vtok.sub(mc * 1024 + (2 * hd + dc) * 128, mc * 1024 + (2 * hd + dc + 1) * 128)
                            pm = pv.sub(mc * T, (mc + 1) * T)
                            mm(bO, bO.ap, vv, vv.ap, pm, pm.ap, mc == 0, mc == 1)
                        dv = yT.sub((16 + 2 * hd + dc) * T, (17 + 2 * hd + dc) * T)
                        tt("dve", dv, dv.ap, bO, bO.ap, rv, rv.ap, ALU.mult)

                for mg in range(4):
                    for n in range(3):
                        par = (mg * 3 + n) % 2
                        w = load_win(12 + n * 4 + mg)
                        for cc in range(4):
                            dv = sgt.sub((par * 4 + cc) * T, (par * 4 + cc + 1) * T)
                            proj_fm(w, cc, lambda bk, dv=dv: act(dv, dv.ap, bk, bk.ap, AF.Sigmoid))
                        wb = wbr_s[par]
                        dma(wb, DR(s_wbr[n * 4 + mg]), "wbr%d" % par)
                        for cc in range(4):
                            bk = bank()
                            for k in range(8):
                                wk = wb.sub(k * 512 + cc * 128, k * 512 + (cc + 1) * 128)
                                yk = yT.sub((n * 8 + k) * T, (n * 8 + k + 1) * T)
                                mm(bk, bk.ap, wk, wk.ap, yk, yk.ap, k == 0, k == 7)
                            sg_ = sgt.sub((par * 4 + cc) * T, (par * 4 + cc + 1) * T)
                            acc = mT.sub((mg * 4 + cc) * T, (mg * 4 + cc + 1) * T)
                            if n == 0:
                                tt("dve", acc, acc.ap, bk, bk.ap, sg_, sg_.ap, ALU.mult)
                            else:
                                tm = mtmp.sub((cc % 2) * T, (cc % 2 + 1) * T)
                                tt("dve", tm, tm.ap, bk, bk.ap, sg_, sg_.ap, ALU.mult)
                                tt("pool", acc, acc.ap, acc, acc.ap, tm, tm.ap, ALU.add)

                for cg in range(4):
                    w = win_s[wctr2[0] % 2]
                    sem = "win%d" % (wctr2[0] % 2)
                    wctr2[0] += 1
                    dma(w, DR(s_wout[cg]), sem)
                    for b in range(NB):
                        bk = bank()
                        for k in range(16):
                            mk = mT.sub(k * T + b * 128, k * T + (b + 1) * 128)
                            wk = w.sub(k * 512, (k + 1) * 512)
                            mm(bk, bk.ap, mk, mk.ap, wk, wk.ap, k == 0, k == 15)
                        xv = xs.sub(b * D + cg * 512, b * D + (cg + 1) * 512)
                        tt("dve", xv, xv.ap, bk, bk.ap, xv, xv.ap, ALU.add)

                if stop == "mixer":
                    dump("x1", xs, [128, NB * D])
                    dump("yT", yT, [128, 24 * T], BF16)
                    dump("mT", mT, [128, 16 * T], BF16)
                    dump("hT", hT, [128, 16 * T], BF16)
                checkpoint("mixer")
                norm_transpose(xs, NB, hT, T, hbs, junk)
                for cg in range(4):
                    w = win_s[wctr2[0] % 2]
                    sem = "win%d" % (wctr2[0] % 2)
                    wctr2[0] += 1
                    dma(w, DR(s_wq[cg]), sem)
                    for cc in range(4):
                        hp = cg * 4 + cc
                        dv = q2T.sub(hp * T, (hp + 1) * T)
                        proj_fm(w, cc, lambda bk, dv=dv: tcopy(rr_eng(["act", "dve"]), dv, dv.ap, bk, bk.ap))
                dma(keysT, DR(s_small[:, 5120:7168]), "keysT")
                for b in range(NB):
                    for q4 in range(4):
                        bk = bank()
                        for c4 in range(4):
                            hp = q4 * 4 + c4
                            qq = q2T.sub(hp * T + b * 128, hp * T + (b + 1) * 128)
                            kk = keysT.sub(hp * 128, (hp + 1) * 128)
                            mm(bk, bk.ap[:, c4 * 128:(c4 + 1) * 128], qq, qq.ap, kk, kk.ap, True, True)
                        dv = sc.sub(b * 2048 + q4 * 512, b * 2048 + (q4 + 1) * 512)
                        tcopy(rr_eng(["act", "dve"]), dv, dv.ap, bk, bk.ap)
                NR = NEXP // 512
                wdn = [0]
                wun = [0]
                wd_piece = {}
                wu_piece = {}

                def load_wd(p):
                    if p >= 2 * NR or p in wd_piece:
                        return
                    slot = p % 3
                    dma(wd_s[slot], DR(s_wdT[p]), "wd%d" % slot)
                    wd_piece[p] = wd_s[slot]

                def load_wu(p):
                    if p >= 4 * NR or p in wu_piece:
                        return
                    slot = p % 4
                    dma(wu_s[slot], DR(s_wup[p]), "wu%d" % slot)
                    wu_piece[p] = wu_s[slot]

                dbk = [0]
                ubk = [0]

                def emit_down(r, cc):
                    half, c2 = cc // 2, cc % 2
                    p = r * 2 + half
                    if c2 == 0:
                        load_wd(p)
                        load_wd(p + 1)
                        load_wd(p + 2)
                    wd = wd_piece[p]
                    bk = bankx(2 + dbk[0] % 2)
                    dbk[0] += 1
                    for k in range(16):
                        wk = wd.sub(k * 256 + c2 * 128, k * 256 + (c2 + 1) * 128)
                        mm(bk, bk.ap, wk, wk.ap, hT_k(k), hT_k(k).ap, k == 0, k == 15)
                    dv = gd2[r % GDN].sub(cc * T, (cc + 1) * T)
                    act(dv, dv.ap, bk, bk.ap, AF.Gelu_apprx_tanh)

                cctr = [0]
                chain_slot = {}

                def emit_chainA(r, b):
                    sl = cctr[0] % NCH
                    cctr[0] += 1
                    chain_slot[(r, b)] = sl
                    Sg, Wg = Sg2[sl], Wg2[sl]
                    Eg = Sg
                    scb = sc.sub(b * 2048, (b + 1) * 2048)
                    s4 = scb.ap.rearrange("p (h t k) -> p h t k", h=8, t=2)
                    in0 = s4[:, :, 0, 4 * r:4 * r + 4].unsqueeze(3).to_broadcast([128, 8, 4, 128])
                    in1 = s4[:, :, 1, :].unsqueeze(2).to_broadcast([128, 8, 4, 128])
                    S.op("dve", lambda e, o=Sg.ap.rearrange("p (h i j) -> p h i j", h=8, i=4), a=in0, c=in1:
                         e.tensor_tensor(out=o, in0=a, in1=c, op=ALU.add), reads=[scb], writes=[Sg])
                    act(Eg, Eg.ap, Sg, Sg.ap, AF.Prelu, alpha=1.0e5)
                    act(Wg, Wg.ap, Eg, Eg.ap, AF.Exp)

                def emit_chainB(r, b):
                    pass

                def emit_gt(r, b):
                    Wg = Wg2[chain_slot[(r, b)]]
                    gbk = bankx(b % 2)
                    for cc in range(4):
                        for h in range(8):
                            wv = Wg.sub(h * 512 + cc * 128, h * 512 + (cc + 1) * 128)
                            dv = dgm.sub((b * 8 + h) * 128, (b * 8 + h + 1) * 128)
                            mm(gbk, gbk.ap[:, cc * 128:(cc + 1) * 128], wv, wv.ap, dv, dv.ap, h == 0, h == 7)
                    at = AT2[r % 2]
                    gdv = gd2[r % GDN]
                    oap = at.ap.rearrange("p (c s) -> p c s", s=T)[:, :, b * 128:(b + 1) * 128]
                    gap = gdv.ap.rearrange("p (c s) -> p c s", s=T)[:, :, b * 128:(b + 1) * 128]
                    tt("dve", at.span(b * 128, 3 * T + (b + 1) * 128, oap), oap, gbk, gbk.ap.rearrange("p (c s) -> p c s", s=128),
                       gdv.span(b * 128, 3 * T + (b + 1) * 128, gap), gap, ALU.mult)

                def emit_up(r, q):
                    cg = q
                    p = r * 4 + cg
                    load_wu(p)
                    load_wu(p + 1)
                    load_wu(p + 2)
                    load_wu(p + 3)
                    wu = wu_piece[p]
                    at = AT2[r % 2]
                    for b in range(NB):
                        bk = bankx(4 + ubk[0] % 4)
                        ubk[0] += 1
                        for cc in range(4):
                            av = at.sub(cc * T + b * 128, cc * T + (b + 1) * 128)
                            wv = wu.sub(cc * 512, (cc + 1) * 512)
                            mm(bk, bk.ap, av, av.ap, wv, wv.ap, cc == 0, cc == 3)
                        xv = xs.sub(b * D + cg * 512, b * D + (cg + 1) * 512)
                        tt("dve", xv, xv.ap, bk, bk.ap, xv, xv.ap, ALU.add)

                load_wd(0)
                load_wd(1)
                for r_ in range(GDN - 1):
                    for cc in range(4):
                        emit_down(r_, cc)
                AXX = mybir.AxisListType.X
                for b in range(NB):
                    svs = [sc.sub(b * 2048 + hp * 128, b * 2048 + (hp + 1) * 128) for hp in range(16)]
                    tps = [top.sub((b * 16 + hp) * 16, (b * 16 + hp + 1) * 16) for hp in range(16)]
                    wks = [cand8.sub(hp * 128, (hp + 1) * 128) for hp in range(16)]
                    for hp in range(16):
                        S.op("dve", lambda e, o=tps[hp].ap[:, 0:8], i=svs[hp].ap: e.max(out=o, in_=i), reads=[svs[hp]], writes=[tps[hp].sub(0, 8)])
                    for hp in range(16):
                        S.op("dve", lambda e, o=wks[hp].ap, r=tps[hp].ap[:, 0:8], i=svs[hp].ap: e.match_replace(out=o, in_to_replace=r, in_values=i, imm_value=NEG),
                             reads=[svs[hp], tps[hp].sub(0, 8)], writes=[wks[hp]])
                    for hp in range(16):
                        S.op("dve", lambda e, o=tps[hp].ap[:, 8:16], i=wks[hp].ap: e.max(out=o, in_=i), reads=[wks[hp]], writes=[tps[hp].sub(8, 16)])
                    tb = top.sub(b * 256, (b + 1) * 256)
                    scb_ = sc.sub(b * 2048, (b + 1) * 2048)
                    tt("dve", penb, penb.ap.rearrange("p (q k) -> p q k", k=128), scb_, scb_.ap.rearrange("p (q k) -> p q k", k=128),
                       tb, tb.ap.rearrange("p (q k) -> p q k", k=16)[:, :, 15:16].to_broadcast([128, 16, 128]), ALU.is_lt)
                    stt(scb_, scb_.ap, penb, penb.ap, -1.0e4, scb_, scb_.ap, ALU.mult, ALU.add)
                    t4 = tb.ap.rearrange("p (h t k) -> p h t k", h=8, t=2)
                    tt("pool", cand8, cand8.ap.rearrange("p (h a c) -> p h a c", h=8, a=16), tb, t4[:, :, 0, :].unsqueeze(3).to_broadcast([128, 8, 16, 16]),
                       tb, t4[:, :, 1, :].unsqueeze(2).to_broadcast([128, 8, 16, 16]), ALU.add)
                    cas = [cand8.sub(h * 256, (h + 1) * 256) for h in range(8)]
                    c24s = [c24.sub(h * 24, (h + 1) * 24) for h in range(8)]
                    for rnd in range(3):
                        for h in range(8):
                            S.op("dve", lambda e, o=c24s[h].ap[:, rnd * 8:rnd * 8 + 8], i=cas[h].ap: e.max(out=o, in_=i),
                                 reads=[cas[h]], writes=[c24s[h].sub(rnd * 8, rnd * 8 + 8)])
                        if rnd < 2:
                            for h in range(8):
                                S.op("dve", lambda e, o=cas[h].ap, r=c24s[h].ap[:, rnd * 8:rnd * 8 + 8]: e.match_replace(out=o, in_to_replace=r, in_values=o, imm_value=NEG),
                                     reads=[cas[h], c24s[h].sub(rnd * 8, rnd * 8 + 8)], writes=[cas[h]])
                    c3 = c24.ap.rearrange("p (h k) -> p h k", k=24)
                    thr = pst.sub(b * 8, b * 8 + 8)
                    lz = pst.sub(32 + b * 8, 32 + b * 8 + 8)
                    cbv = pst.sub(64 + b * 8, 64 + b * 8 + 8)
                    tt("dve", thr, thr.ap, c24, c3[:, :, 15], c24, c3[:, :, 16], ALU.add)
                    ts("dve", thr, thr.ap, thr, thr.ap, 0.5, ALU.mult)
                    d16 = candb.sub(0, 128)
                    tt("dve", d16, d16.ap.rearrange("p (h k) -> p h k", k=16), c24, c3[:, :, 0:16], c24, c3[:, :, 0:1].to_broadcast([128, 8, 16]), ALU.subtract)
                    act(d16, d16.ap, d16, d16.ap, AF.Exp)
                    S.op("dve", lambda e, o=lz.ap, i=d16.ap.rearrange("p (h k) -> p h k", k=16): e.tensor_reduce(out=o, in_=i, axis=AXX, op=ALU.add),
                         reads=[d16], writes=[lz])
                    act(lz, lz.ap, lz, lz.ap, AF.Ln)
                    tt("dve", cbv, cbv.ap, thr, thr.ap, c24, c3[:, :, 0], ALU.subtract)
                    tt("dve", cbv, cbv.ap, cbv, cbv.ap, lz, lz.ap, ALU.subtract)
                    ev = ecb.sub(b * 8, b * 8 + 8)
                    act(ev, ev.ap, cbv, cbv.ap, AF.Exp)
                    scb = sc.sub(b * 2048, (b + 1) * 2048)
                    s0ap = scb.ap.rearrange("p (h t k) -> p h t k", h=8, t=2)[:, :, 0, :]
                    tt("dve", scb.span(0, 2048, s0ap), s0ap, scb.span(0, 2048, s0ap), s0ap, thr, thr.ap.unsqueeze(2).to_broadcast([128, 8, 128]), ALU.subtract)
                    dv = dgm.sub(b * 1024, (b + 1) * 1024)
                    tt("pool", dv, dv.ap.rearrange("p (h s) -> p h s", s=128), identb, identb.ap.unsqueeze(1).to_broadcast([128, 8, 128]),
                       ev, ev.ap.unsqueeze(2).to_broadcast([128, 8, 128]), ALU.mult)
                if stop == "topk":
                    dump("sc", sc, [128, NB * 2048])
                    dump("top", top, [128, NB * 256])
                    dump("pst", pst, [128, NB * 32])
                    dump("ecb", ecb, [128, NB * 8])
                    dump("hT", hT, [128, 16 * T], BF16)
                checkpoint("topk")

                load_wu(0)
                load_wu(1)
                def blk(n):
                    return (n // NB, n % NB)

                NBLK = NR * NB
                for n0 in range(NCH):
                    emit_chainA(*blk(n0))
                for n in range(NBLK):
                    r, b = blk(n)
                    emit_gt(r, b)
                    if n + NCH < NBLK:
                        emit_chainA(*blk(n + NCH))
                    if r + GDN - 1 < NR and b % 2 == 0:
                        emit_down(r + GDN - 1, b)
                        emit_down(r + GDN - 1, b + 1)
                    if r >= 1:
                        emit_up(r - 1, b)
                for q in range(4):
                    emit_up(NR - 1, q)

                nfg = View(abuf, Sg2[0].lo, Sg2[0].lo + D * 4, A[:, Sg2[0].lo // 4:Sg2[0].lo // 4 + D], F32)
                dma(nfg, DR(gf_d.partition_broadcast(128)), "nfg")
                for b in range(NB):
                    xb = xs.sub(b * D, (b + 1) * D)
                    ss = stat.sub(16 + b, 17 + b)
                    rs = stat.sub(24 + b, 25 + b)
                    jk = Wg2[0].sub(0, D)
                    memset("pool", ss, ss.ap, 0.0)
                    act(jk, jk.ap, xb, xb.ap, AF.Square, extra_r=[ss], extra_w=[ss], accum_out=ss.ap)
                    ts("dve", rs, rs.ap, ss, ss.ap, 1.0 / D, ALU.mult, EPS, ALU.add)
                    act(rs, rs.ap, rs, rs.ap, AF.Sqrt)
                    S.op("dve", lambda e, o=rs.ap: e.reciprocal(out=o, in_=o), reads=[rs], writes=[rs])
                    osrc = (Sg2[1], Wg2[1])[b % 2]
                    ost = View(abuf, osrc.lo, osrc.lo + D * 4, A[:, osrc.lo // 4:osrc.lo // 4 + D], F32)
                    stt(ost, ost.ap, xb, xb.ap, rs.ap, nfg, nfg.ap, ALU.mult, ALU.mult, extra_r=[rs])
                    dma(View(ybuf, r0 + b * 128, r0 + (b + 1) * 128, y_d[r0 + b * 128:r0 + (b + 1) * 128, :]), ost, "ys%d" % (b % 2))

        except _Stop:
            pass
        S.dma_barrier("sp")
        S.emit(st)
        info = dict(nops=len(S.ops), nwait=S.nwait, nsem=S.nsem, arena=off[0])
    return nc, info


_CACHE = {}


def _prep_shared(inputs):
    f = lambda a: np.ascontiguousarray(np.asarray(a, dtype=np.float32))
    sh = {
        "norm_mix_g": f(inputs["norm_mix_g"]).reshape(D),
        "norm_mem_g": f(inputs["norm_mem_g"]).reshape(D),
        "w_in": f(inputs["w_in"]).reshape(D, DIN),
        "gmlp_w_s": f(inputs["gmlp_w_s"]).reshape(8, 128, 128),
        "gmlp_b_s": f(inputs["gmlp_b_s"]).reshape(8 * 128),
        "gmlp_ln_g": f(inputs["gmlp_ln_g"]).reshape(1024),
        "gmlp_ln_b": f(inputs["gmlp_ln_b"]).reshape(1024),
        "conv_w": f(inputs["conv_w"]).reshape(3, 1024),
        "w_kv": f(inputs["w_kv"]).reshape(D, 2048),
        "w_branch": f(inputs["w_branch"]).reshape(3, 1024, D),
        "w_out": f(inputs["w_out"]).reshape(D, D),
        "norm_ffn_g": f(inputs["norm_ffn_g"]).reshape(D),
        "peer_w_q": f(inputs["peer_w_q"]).reshape(D, 2048),
        "peer_keys": f(inputs["peer_keys"]).reshape(16, 128, 128),
        "peer_w_down": f(inputs["peer_w_down"]).reshape(NEXP, D),
        "peer_w_up": f(inputs["peer_w_up"]).reshape(NEXP, D),
        "norm_final_g": f(inputs["norm_final_g"]).reshape(D),
        "ident": np.eye(128, dtype=np.float32),
    }
    return sh


def kernel(**inputs):
    x = np.asarray(inputs["x"], dtype=np.float32)
    mem = np.asarray(inputs["mem"], dtype=np.float32)
    B, Sq, _ = x.shape
    ncores = 8
    per = (B * Sq) // ncores
    halves = Sq // per
    if "nc" not in _CACHE:
        _CACHE["nc"] = build(ntiles=per // T)[0]
    nc = _CACHE["nc"]
    sh = _prep_shared(inputs)
    in_maps = []
    for c in range(ncores):
        b, hf = c // halves, c % halves
        s0 = hf * per
        xh = np.zeros((2, D), np.float32)
        if s0 > 0:
            xh[:] = x[b, s0 - 2:s0]
        m = dict(sh)
        m["x"] = np.ascontiguousarray(x[b, s0:s0 + per])
        m["xh"] = xh
        m["mem"] = np.ascontiguousarray(mem[b])
        in_maps.append(m)
    res = run_bass_kernel_spmd(nc, in_maps, core_ids=list(range(ncores)))
    out = np.empty((B, Sq, D), np.float32)
    for c in range(ncores):
        b, hf = c // halves, c % halves
        out[b, hf * per:(hf + 1) * per] = res.results[c]["y"]
    return out
```

```python
import numpy as np
from contextlib import ExitStack
import concourse.bass as bass
import concourse.mybir as mybir
from concourse.bass_utils import run_bass_kernel_spmd

F32 = mybir.dt.float32
BF16 = mybir.dt.bfloat16
AF = mybir.ActivationFunctionType
ALU = mybir.AluOpType
DT_SIZE = {F32: 4, BF16: 2}

P = 128
D = 2048
DIN = 12288
T = 512
NB = 4
NEXP = 16384
NTOK_CORE = 4096
EPS = 1e-6
NEG = -1.0e30


class View:
    __slots__ = ("buf", "lo", "hi", "ap", "dtype", "dense")

    def __init__(self, buf, lo, hi, ap, dtype=None, dense=True):
        self.buf = buf
        self.lo = lo
        self.hi = hi
        self.ap = ap
        self.dtype = dtype
        self.dense = dense

    def sub(self, a, b):
        sz = DT_SIZE[self.dtype]
        return View(self.buf, self.lo + a * sz, self.lo + b * sz, self.ap[:, a:b], self.dtype, True)

    def span(self, a, b, ap):
        sz = DT_SIZE[self.dtype]
        return View(self.buf, self.lo + a * sz, self.lo + b * sz, ap, self.dtype, False)


class Buf:
    def __init__(self, name, handle=None, excl=False):
        self.name = name
        self.h = handle
        self.excl = excl
        self.last = []
        self.writers = []
        self.readers = {}


class Sched:
    def __init__(self, nc):
        self.nc = nc
        self.ops = []
        self.engobj = {"pe": nc.tensor, "act": nc.scalar, "dve": nc.vector,
                       "pool": nc.gpsimd, "sp": nc.sync}

    def _deps_for(self, reads, writes, opid, engkey):
        deps = set()
        ex = {}
        for v in reads:
            if v.buf.excl:
                ex.setdefault(id(v.buf), [v.buf, False])
        for v in writes:
            if v.buf.excl:
                ex.setdefault(id(v.buf), [v.buf, False])[1] = True
        for b, isw in ex.values():
            for (o, pw) in b.last:
                deps.add((o, "raw" if (pw and not isw) else "waw"))
            b.last = [(opid, isw)]
        reads = [v for v in reads if not v.buf.excl]
        writes = [v for v in writes if not v.buf.excl]
        for v in reads:
            for (lo, hi, o, _d) in v.buf.writers:
                if lo < v.hi and v.lo < hi:
                    deps.add((o, "raw"))
        for v in writes:
            b = v.buf
            for (lo, hi, o, _d) in b.writers:
                if lo < v.hi and v.lo < hi:
                    deps.add((o, "waw"))
            for (lo, hi, _k), o in b.readers.items():
                if lo < v.hi and v.lo < hi:
                    deps.add((o, "war"))
        for v in writes:
            b = v.buf
            if v.dense:
                b.writers = [w for w in b.writers if not (v.lo <= w[0] and w[1] <= v.hi)]
                b.readers = {k: o for k, o in b.readers.items() if not (v.lo <= k[0] and k[1] <= v.hi)}
            else:
                b.writers = [w for w in b.writers if not (w[0] == v.lo and w[1] == v.hi and not w[3])]
            b.writers.append((v.lo, v.hi, opid, v.dense))
        for v in reads:
            v.buf.readers[(v.lo, v.hi, engkey)] = opid
        return deps

    def op(self, eng, fn, reads=(), writes=(), dma_sem=None):
        opid = len(self.ops)
        engkey = eng if dma_sem is None else ("dma", dma_sem)
        deps = self._deps_for(reads, writes, opid, engkey)
        self.ops.append(dict(eng=eng, fn=fn, deps=deps, dma_sem=dma_sem, signal=False, barrier=False))
        return opid

    def dma_barrier(self, eng="sp"):
        self.ops.append(dict(eng=eng, fn=None, deps=set(), dma_sem=None, signal=False, barrier=True))

    def emit(self, stack):
        nc = self.nc
        ops = self.ops
        for o in ops:
            need = set()
            for (p, kind) in o["deps"]:
                po = ops[p]
                if po["dma_sem"] is None and po["eng"] == o["eng"] and o["dma_sem"] is None:
                    if o["eng"] == "pe":
                        continue
                need.add(p)
            latest = {}
            keep = set()
            for p in need:
                po = ops[p]
                if po["dma_sem"] is not None:
                    keep.add(p)
                elif po["eng"] not in latest or p > latest[po["eng"]]:
                    latest[po["eng"]] = p
            keep.update(latest.values())
            o["need"] = keep
            for p in keep:
                ops[p]["signal"] = True
        sems = {}
        for e in ("pe", "act", "dve", "pool"):
            sems[e] = stack.enter_context(nc.semaphore("s_" + e))
        for o in ops:
            if o["dma_sem"] is not None:
                k = ("dma", o["dma_sem"])
                if k not in sems:
                    sems[k] = stack.enter_context(nc.semaphore("d_" + str(o["dma_sem"])))
        count = {k: 0 for k in sems}
        waited = {e: {} for e in self.engobj}
        nwait = 0
        for o in ops:
            e = o["eng"]
            eo = self.engobj[e]
            if o["barrier"]:
                for sk, val in count.items():
                    if isinstance(sk, tuple) and val > 0 and waited[e].get(sk, 0) < val:
                        eo.wait_ge(sems[sk], val)
                        waited[e][sk] = val
                        nwait += 1
                continue
            tg = {}
            for p in o["need"]:
                po = ops[p]
                sk, val = po["sig"]
                if po["dma_sem"] is not None:
                    val = count[sk]
                if tg.get(sk, 0) < val:
                    tg[sk] = val
            for sk, val in tg.items():
                if waited[e].get(sk, 0) >= val:
                    continue
                eo.wait_ge(sems[sk], val)
                waited[e][sk] = val
                nwait += 1
            ins = o["fn"](eo)
            if o["dma_sem"] is not None:
                sk = ("dma", o["dma_sem"])
                count[sk] += 16
                ins.then_inc(sems[sk], 16)
                o["sig"] = (sk, count[sk])
            elif o["signal"]:
                count[e] += 1
                ins.then_inc(sems[e], 1)
                o["sig"] = (e, count[e])
            else:
                o["sig"] = None
        self.nwait = nwait
        self.nsem = len(sems)
        self.counts = count


class _Stop(Exception):
    pass


def build(ntiles=8, debug=False, stop=None):
    nc = bass.Bass("TRN2", target_bir_lowering=False)
    ntok = ntiles * T

    def din(name, shape):
        return nc.dram_tensor(name, list(shape), F32, kind="ExternalInput").ap()

    x_d = din("x", [ntok, D])
    xh_d = din("xh", [2, D])
    mem_d = din("mem", [256, D])
    g1_d = din("norm_mix_g", [D])
    gm_d = din("norm_mem_g", [D])
    win_d = din("w_in", [D, DIN])
    ws_d = din("gmlp_w_s", [8, 128, 128])
    bs_d = din("gmlp_b_s", [8 * 128])
    lng_d = din("gmlp_ln_g", [1024])
    lnb_d = din("gmlp_ln_b", [1024])
    cw_d = din("conv_w", [3, 1024])
    wkv_d = din("w_kv", [D, 2048])
    wbr_d = din("w_branch", [3, 1024, D])
    wout_d = din("w_out", [D, D])
    g2_d = din("norm_ffn_g", [D])
    wq_d = din("peer_w_q", [D, 2048])
    keys_d = din("peer_keys", [16, 128, 128])
    wdn_d = din("peer_w_down", [NEXP, D])
    wup_d = din("peer_w_up", [NEXP, D])
    gf_d = din("norm_final_g", [D])
    ident_d = din("ident", [128, 128])
    y_d = nc.dram_tensor("y", [ntok, D], F32, kind="ExternalOutput").ap()

    def dscr(name, shape):
        return nc.dram_tensor(name, list(shape), BF16, kind="Internal").ap()

    s_win = dscr("s_win", [24, 128, 16, 512])
    s_wbr = dscr("s_wbr", [12, 128, 8, 512])
    s_wout = dscr("s_wout", [4, 128, 16, 512])
    s_wq = dscr("s_wq", [4, 128, 16, 512])
    s_wdT = dscr("s_wdT", [64, 128, 16, 256])
    s_wup = dscr("s_wup", [128, 128, 4, 512])
    s_small = dscr("s_small", [128, 7168])

    dbg = {}

    st = ExitStack()
    with st:
        S = Sched(nc)
        ARENA_BYTES = 207 * 1024
        A = st.enter_context(nc.sbuf_tensor("arena", [128, ARENA_BYTES // 4], F32))
        abuf = Buf("arena", A)
        off = [0]

        def alloc(n, dt):
            nb = (n * DT_SIZE[dt] + 3) // 4 * 4
            lo = off[0]
            off[0] += nb
            assert off[0] <= ARENA_BYTES, ("arena overflow", off[0])
            ap = A[:, lo // 4:(lo + nb) // 4]
            if dt != F32:
                ap = ap.bitcast(dt)
            return View(abuf, lo, lo + nb, ap, dt)

        banks = []
        for i in range(8):
            t_ = st.enter_context(nc.psum_tensor("bank%d" % i, [128, 512], F32))
            banks.append(Buf("bank%d" % i, t_, excl=True))
        bctr = [0]

        def bank(dt=F32):
            i = bctr[0] % 8
            bctr[0] += 1
            ap = banks[i].h[:, :]
            if dt != F32:
                ap = ap.bitcast(dt)
            return View(banks[i], 0, 2048, ap, dt)

        rd_only = Buf("dram_ro")
        ybuf = Buf("y")
        scr_w = Buf("scratch_w")
        wctr = [0]

        def DR(ap):
            return View(rd_only, 0, 1, ap, None)

        def DW(ap):
            wctr[0] += 1
            return View(scr_w, wctr[0], wctr[0] + 1, ap, None)

        def dma(outv, inv, sem, eng="sp", **kw):
            S.op(eng, lambda e, o=outv.ap, i=inv.ap, kw=kw: e.dma_start(out=o, in_=i, **kw),
                 reads=[inv], writes=[outv], dma_sem=sem)

        def mm(outv, out_ap, lv, l_ap, rv, r_ap, start, stop):
            S.op("pe", lambda e, o=out_ap, l=l_ap, r=r_ap, a=start, b=stop:
                 e.matmul(o, lhsT=l, rhs=r, start=a, stop=b), reads=[lv, rv], writes=[outv])

        def tr(outv, out_ap, inv, in_ap, idv):
            S.op("pe", lambda e, o=out_ap, i=in_ap, d=idv.ap: e.transpose(out=o, in_=i, identity=d),
                 reads=[inv, idv], writes=[outv])

        def act(outv, out_ap, inv, in_ap, func, extra_r=(), extra_w=(), **kw):
            S.op("act", lambda e, o=out_ap, i=in_ap, f=func, kw=kw: e.activation(out=o, in_=i, func=f, **kw),
                 reads=[inv] + list(extra_r), writes=[outv] + list(extra_w))

        def tcopy(eng, outv, out_ap, inv, in_ap):
            if eng == "act":
                S.op("act", lambda e, o=out_ap, i=in_ap: e.copy(out=o, in_=i), reads=[inv], writes=[outv])
            else:
                S.op(eng, lambda e, o=out_ap, i=in_ap: e.tensor_copy(out=o, in_=i), reads=[inv], writes=[outv])

        def tt(eng, outv, out_ap, av, a_ap, bv, b_ap, op):
            S.op(eng, lambda e, o=out_ap, a=a_ap, b=b_ap, op=op: e.tensor_tensor(out=o, in0=a, in1=b, op=op),
                 reads=[av, bv], writes=[outv])

        def ts(eng, outv, out_ap, inv, in_ap, s1, op0, s2=None, op1=None, extra_r=()):
            def f(e, o=out_ap, i=in_ap, s1=s1, s2=s2, op0=op0, op1=op1):
                if op1 is None:
                    return e.tensor_scalar(out=o, in0=i, scalar1=s1, scalar2=None, op0=op0)
                return e.tensor_scalar(out=o, in0=i, scalar1=s1, scalar2=s2, op0=op0, op1=op1)
            S.op(eng, f, reads=[inv] + list(extra_r), writes=[outv])

        def stt(outv, out_ap, av, a_ap, scalar, bv, b_ap, op0, op1, extra_r=()):
            S.op("dve", lambda e, o=out_ap, a=a_ap, s=scalar, b=b_ap, op0=op0, op1=op1:
                 e.scalar_tensor_tensor(out=o, in0=a, scalar=s, in1=b, op0=op0, op1=op1),
                 reads=[av, bv] + list(extra_r), writes=[outv])

        def memset(eng, v, ap, val):
            S.op(eng, lambda e, a=ap, c=val: e.memset(a, c), writes=[v])

        rr = [0]

        def rr_eng(choices):
            rr[0] += 1
            return choices[rr[0] % len(choices)]

        xs = alloc(NB * D, F32)
        hT = alloc(16 * T, BF16)
        identf = alloc(128, F32)
        identb = alloc(128, BF16)
        onesb = alloc(128, BF16)
        convw = alloc(8 * 3, F32)
        g1f = alloc(16, F32)
        g2f = alloc(16, F32)
        gmf = alloc(16, F32)
        gT = alloc(8 * 516, BF16)
        stat = alloc(64, F32)
        PERSIST = off[0]

        def hT_k(k):
            return hT.sub(k * T, (k + 1) * T)

        dctr = [0]

        def dump(name, srcv, shape, dt=F32):
            o = nc.dram_tensor("dbg_" + name, list(shape), dt, kind="ExternalOutput").ap()
            dctr[0] += 1
            dma(View(ybuf, 10 ** 9 + dctr[0], 10 ** 9 + dctr[0] + 1, o), srcv, "dbg%d" % dctr[0])

        def checkpoint(name):
            if stop == name:
                raise _Stop()

        try:
            dma(identf, DR(ident_d), "c_id")
            tcopy("dve", identb, identb.ap, identf, identf.ap)
            memset("dve", onesb, onesb.ap, 1.0)
            for k_ in range(3):
                dma(convw.sub(k_ * 8, k_ * 8 + 8), DR(cw_d[k_].rearrange("(c p) -> p c", p=128)), "c_cw", allow_slow_non_contiguous=True)
            dma(g1f, DR(g1_d.rearrange("(k p) -> p k", p=128)), "c_g1", allow_slow_non_contiguous=True)
            dma(g2f, DR(g2_d.rearrange("(k p) -> p k", p=128)), "c_g2", allow_slow_non_contiguous=True)
            dma(gmf, DR(gm_d.rearrange("(k p) -> p k", p=128)), "c_gm", allow_slow_non_contiguous=True)

            def norm_transpose(src_v, nblk, dstT, dst_cols, tmp_hb, tmp_junk):
                for b in range(nblk):
                    xb = src_v.sub(b * D, (b + 1) * D)
                    ss = stat.sub(b, b + 1)
                    rs = stat.sub(8 + b, 9 + b)
                    hb = tmp_hb[b % len(tmp_hb)]
                    tmp_junk = hb
                    memset("pool", ss, ss.ap, 0.0)
                    act(tmp_junk, tmp_junk.ap, xb, xb.ap, AF.Square, extra_r=[ss], extra_w=[ss], accum_out=ss.ap)
                    ts("dve", rs, rs.ap, ss, ss.ap, 1.0 / D, ALU.mult, EPS, ALU.add)
                    act(rs, rs.ap, rs, rs.ap, AF.Sqrt)
                    S.op("dve", lambda e, o=rs.ap: e.reciprocal(out=o, in_=o), reads=[rs], writes=[rs])
                    act(hb, hb.ap, xb, xb.ap, AF.Copy, extra_r=[rs], scale=rs.ap)
                    for half in range(2):
                        bk = bank(BF16)
                        for kk in range(8):
                            k = half * 8 + kk
                            tr(bk, bk.ap[:, kk * 128:(kk + 1) * 128], hb, hb.ap[:, k * 128:(k + 1) * 128], identb)
                        a0 = half * 8 * dst_cols + b * 128
                        a1 = (half * 8 + 7) * dst_cols + (b + 1) * 128
                        dap = dstT.ap.rearrange("p (k s) -> p k s", s=dst_cols)[:, half * 8:half * 8 + 8, b * 128:(b + 1) * 128]
                        dv = dstT.span(a0, a1, dap)
                        tcopy(rr_eng(["act", "dve"]), dv, dap, bk, bk.ap.rearrange("p (k s) -> p k s", s=128))

            PRO = off[0]
            NSTG = 4
            stg = [alloc(4096, F32) for _ in range(NSTG)]
            stgb = [alloc(4096, BF16) for _ in range(NSTG)]
            pctr = [0]
            small = alloc(7168, BF16)
            kT = small.sub(0, 2048)
            vtok = small.sub(2048, 4096)
            wsT = small.sub(4096, 5120)
            keysT = small.sub(5120, 7168)

            pieces = []

            def run_pieces():
                n = len(pieces)
                base = pctr[0]
                for j in range(min(NSTG - 1, n)):
                    pieces[j][0]((base + j) % NSTG)
                for i_ in range(n):
                    j = i_ + NSTG - 1
                    if j < n:
                        pieces[j][0]((base + j) % NSTG)
                    pieces[i_][1]((base + i_) % NSTG)
                pctr[0] += n
                del pieces[:]

            def cast_rows(w_ap, nrows, ncols, gain, store_fn):
                cp = min(ncols, 4096)
                for k in range(nrows // 128):
                    for c0 in range(0, ncols, cp):
                        def ld(i, k=k, c0=c0):
                            sv = stg[i].sub(0, cp)
                            dma(sv, DR(w_ap[k * 128:(k + 1) * 128, c0:c0 + cp]), "stg%d" % i)

                        def wk(i, k=k, c0=c0):
                            sv = stg[i].sub(0, cp)
                            bv = stgb[i].sub(0, cp)
                            eng = rr_eng(["dve", "act", "act"])
                            if gain is None:
                                tcopy(eng, bv, bv.ap, sv, sv.ap)
                            elif eng == "act":
                                act(bv, bv.ap, sv, sv.ap, AF.Copy, extra_r=[gain], scale=gain.ap[:, k:k + 1])
                            else:
                                ts(eng, bv, bv.ap, sv, sv.ap, gain.ap[:, k:k + 1], ALU.mult, extra_r=[gain])
                            store_fn(k, c0, cp, bv, "stgb%d" % i)
                        pieces.append((ld, wk))

            def st_win(k, c0, cp, bv, sem):
                g0 = c0 // 512
                ng = cp // 512
                dma(DW(s_win[g0:g0 + ng, :, k, :].rearrange("g p c -> p g c")),
                    View(bv.buf, bv.lo, bv.hi, bv.ap.rearrange("p (g c) -> p g c", c=512)), sem)
            cast_rows(win_d, D, DIN, g1f, st_win)

            for n in range(3):
                def st_wbr(k, c0, cp, bv, sem, n=n):
                    dma(DW(s_wbr[n * 4:(n + 1) * 4, :, k, :].rearrange("g p c -> p g c")),
                        View(bv.buf, bv.lo, bv.hi, bv.ap.rearrange("p (g c) -> p g c", c=512)), sem)
                cast_rows(wbr_d[n], 1024, D, None, st_wbr)

            def st_wout(k, c0, cp, bv, sem):
                dma(DW(s_wout[:, :, k, :].rearrange("g p c -> p g c")),
                    View(bv.buf, bv.lo, bv.hi, bv.ap.rearrange("p (g c) -> p g c", c=512)), sem)
            cast_rows(wout_d, D, D, None, st_wout)

            def st_wq(k, c0, cp, bv, sem):
                dma(DW(s_wq[:, :, k, :].rearrange("g p c -> p g c")),
                    View(bv.buf, bv.lo, bv.hi, bv.ap.rearrange("p (g c) -> p g c", c=512)), sem)
            cast_rows(wq_d, D, 2048, g2f, st_wq)

            def st_wup(k, c0, cp, bv, sem):
                r, cc = k // 4, k % 4
                dma(DW(s_wup[r * 4:(r + 1) * 4, :, cc, :].rearrange("g p c -> p g c")),
                    View(bv.buf, bv.lo, bv.hi, bv.ap.rearrange("p (g c) -> p g c", c=512)), sem)
            run_pieces()

            PRO2 = off[0]
            stgT = [alloc(16 * 512, BF16) for _ in range(2)]
            for eb in range(NEXP // 128):
                def ld(i, eb=eb):
                    sv = stg[i].sub(0, D)
                    dma(sv, DR(wdn_d[eb * 128:(eb + 1) * 128, :]), "stg%d" % i)

                def wk(i, eb=eb):
                    r, cc = eb // 4, eb % 4
                    sv = stg[i].sub(0, D)
                    tv = stgT[r % 2]
                    for q in range(4):
                        bk = bank()
                        for kk in range(4):
                            k = q * 4 + kk
                            tr(bk, bk.ap[:, kk * 128:(kk + 1) * 128], sv, sv.ap[:, k * 128:(k + 1) * 128], identf)
                        eng = rr_eng(["dve", "act"])
                        for kk in range(4):
                            k = q * 4 + kk
                            a0 = k * 512 + cc * 128
                            dv = tv.sub(a0, a0 + 128)
                            src = View(bk.buf, kk * 512, kk * 512 + 512, bk.ap[:, kk * 128:(kk + 1) * 128], F32)
                            if eng == "act":
                                act(dv, dv.ap, src, src.ap, AF.Copy, extra_r=[g2f], scale=g2f.ap[:, k:k + 1])
                            else:
                                ts("dve", dv, dv.ap, src, src.ap, g2f.ap[:, k:k + 1], ALU.mult, extra_r=[g2f])
                    if cc == 3:
                        for half in range(2):
                            sap = tv.ap.rearrange("p (k e) -> p k e", e=512)[:, :, half * 256:(half + 1) * 256]
                            dma(DW(s_wdT[r * 2 + half]), tv.span(0, 16 * 512, sap), "stgT%d" % (r % 2))
                pieces.append((ld, wk))
            run_pieces()

            ktmp = stg[0].sub(0, 128)
            for hp in range(16):
                kv_ = stg[hp % 2].sub(0, 128)
                dma(kv_, DR(keys_d[hp]), "stg%d" % (hp % 2))
                bk = bank()
                tr(bk, bk.ap[:, 0:128], kv_, kv_.ap, identf)
                dv = keysT.sub(hp * 128, (hp + 1) * 128)
                tcopy("dve", dv, dv.ap, View(bk.buf, 0, 512, bk.ap[:, 0:128], F32), bk.ap[:, 0:128])
            for g in range(8):
                wv_ = stg[g % 2].sub(0, 128)
                dma(wv_, DR(ws_d[g]), "stg%d" % (g % 2))
                bk = bank()
                tr(bk, bk.ap[:, 0:128], wv_, wv_.ap, identf)
                dv = wsT.sub(g * 128, (g + 1) * 128)
                tcopy("dve", dv, dv.ap, View(bk.buf, 0, 512, bk.ap[:, 0:128], F32), bk.ap[:, 0:128])
                memset("dve", dv, dv.ap[64:128, 0:64], 0.0)

            off[0] = PRO2
            memx = alloc(2 * D, F32)
            memT = alloc(16 * 256, BF16)
            hb_m = [alloc(D, BF16)]
            junk_m = None
            wkb = [alloc(1024, BF16) for _ in range(2)]
            for b in range(2):
                dma(memx.sub(b * D, (b + 1) * D), DR(mem_d[b * 128:(b + 1) * 128, :]), "memx")
            norm_transpose(memx, 2, memT, 256, hb_m, junk_m)
            for part in range(2):
                accs = [bank() for _ in range(8 if part == 0 else 4)]
                for k in range(16):
                    i = pctr[0] % NSTG
                    pctr[0] += 1
                    sv = stg[i].sub(0, 1024)
                    dma(sv, DR(wkv_d[k * 128:(k + 1) * 128, part * 1024:(part + 1) * 1024]), "stg%d" % i)
                    wb = wkb[k % 2]
                    ts("dve", wb, wb.ap, sv, sv.ap, gmf.ap[:, k:k + 1], ALU.mult, extra_r=[gmf])
                    mk = memT.sub(k * 256, (k + 1) * 256)
                    if part == 0:
                        for c in range(8):
                            mm(accs[c], accs[c].ap[:, 0:256], wb, wb.ap[:, c * 128:(c + 1) * 128], mk, mk.ap, k == 0, k == 15)
                    else:
                        for mc in range(2):
                            for cg in range(2):
                                a = accs[mc * 2 + cg]
                                mm(a, a.ap, mk, mk.ap[:, mc * 128:(mc + 1) * 128], wb, wb.ap[:, cg * 512:(cg + 1) * 512], k == 0, k == 15)
                if part == 0:
                    for c in range(8):
                        dv = kT.sub(c * 256, (c + 1) * 256)
                        tcopy(rr_eng(["act", "dve"]), dv, dv.ap, accs[c], accs[c].ap[:, 0:256])
                else:
                    for mc in range(2):
                        for cg in range(2):
                            dv = vtok.sub(mc * 1024 + cg * 512, mc * 1024 + (cg + 1) * 512)
                            tcopy(rr_eng(["act", "dve"]), dv, dv.ap, accs[mc * 2 + cg], accs[mc * 2 + cg].ap)

            dma(DW(s_small), small, "small_st")
            S.dma_barrier("sp")
            if stop == "pro":
                dump("kT", kT, [128, 8 * 256], BF16)
                dump("vtok", vtok, [128, 2048], BF16)
                dump("keysT", keysT, [128, 2048], BF16)
                dump("wsT", wsT, [128, 1024], BF16)
                dump("memT", memT, [128, 16 * 256], BF16)
                dump("s_win0", DR(s_win[0]), [128, 16, 512], BF16)
                dump("s_win23", DR(s_win[23]), [128, 16, 512], BF16)
                dump("s_wbr5", DR(s_wbr[5]), [128, 8, 512], BF16)
                dump("s_wout1", DR(s_wout[1]), [128, 16, 512], BF16)
                dump("s_wq2", DR(s_wq[2]), [128, 16, 512], BF16)
                dump("s_wdT3", DR(s_wdT[3]), [128, 16, 256], BF16)
            checkpoint("pro")
            off[0] = PRO

            MIX = off[0]
            win_s = [alloc(16 * 512, BF16) for _ in range(2)]
            wctr2 = [0]
            yT = alloc(24 * T, BF16)
            sgt = alloc(8 * T, BF16)
            wbr_s = [alloc(8 * 512, BF16) for _ in range(2)]
            hbs = [alloc(D, BF16) for _ in range(2)]
            junk = None
            msmall = alloc(5120, BF16)
            kT = msmall.sub(0, 2048)
            vtok = msmall.sub(2048, 4096)
            wsT = msmall.sub(4096, 5120)
            bsb = alloc(8 * 128, F32)
            SCR = off[0]
            uT = alloc(8 * T, BF16)
            vtm = alloc(NB * 1024, F32)
            vln = alloc(NB * 1024, BF16)
            lng = alloc(1024, F32)
            lnb = alloc(1024, F32)
            bnst = alloc(16, F32)
            tmpA = alloc(T, F32)
            SCR_END = off[0]
            off[0] = SCR
            cT = alloc(8 * T, BF16)
            tcv = alloc(2 * T, F32)
            off[0] = SCR
            qT = alloc(8 * T, BF16)
            pT = alloc(2 * 2 * T, BF16)
            rD = alloc(2 * T, F32)
            off[0] = SCR
            mT = alloc(16 * T, BF16)
            mtmp = alloc(2 * T, F32)
            assert off[0] <= SCR_END
            off[0] = SCR_END
            MIX_END = off[0]

            off[0] = MIX
            sc = alloc(NB * 16 * 128, F32)
            c24 = alloc(8 * 24, F32)
            pst = alloc(NB * 8 * 4, F32)
            ecb = alloc(NB * 8, F32)
            dgm = alloc(NB * 8 * 128, BF16)
            wd_s = [alloc(16 * 256, BF16) for _ in range(3)]
            wu_s = [alloc(4 * 512, BF16) for _ in range(4)]
            GA = off[0]
            GDN = 6
            gd2 = [alloc(4 * T, BF16) for _ in range(GDN)]
            ATB = off[0]
            AT2 = [alloc(4 * T, BF16) for _ in range(2)]
            GA_END = off[0]
            off[0] = ATB
            top = alloc(NB * 16 * 16, F32)
            candb = alloc(2 * 256, F32)
            assert off[0] <= GA_END
            off[0] = GA_END
            GRID = off[0]
            NCH = 2
            Sg2 = [alloc(4096, BF16) for _ in range(NCH)]
            Wg2 = [alloc(4096, BF16) for _ in range(NCH)]
            GRID_END = off[0]
            off[0] = GRID
            q2T = alloc(16 * T, BF16)
            keysT = alloc(16 * 128, BF16)
            cand8 = alloc(8 * 256, F32)
            penb = alloc(16 * 128, BF16)
            assert off[0] <= GRID_END
            PEER_END = GRID_END
            total_bytes = max(MIX_END, PEER_END)
            assert total_bytes <= ARENA_BYTES, total_bytes

            def bankx(i, dt=F32):
                ap = banks[i].h[:, :]
                if dt != F32:
                    ap = ap.bitcast(dt)
                return View(banks[i], 0, 2048, ap, dt)

            def load_win(g):
                w = win_s[wctr2[0] % 2]
                sem = "win%d" % (wctr2[0] % 2)
                wctr2[0] += 1
                dma(w, DR(s_win[g]), sem)
                return w

            def proj_fm(w, cc, evac):
                bk = bank()
                for k in range(16):
                    wk = w.sub(k * 512 + cc * 128, k * 512 + (cc + 1) * 128)
                    mm(bk, bk.ap, wk, wk.ap, hT_k(k), hT_k(k).ap, k == 0, k == 15)
                evac(bk)

            def conv_gate_cols(ncols, dst_col0):
                for gi in (6, 7):
                    w = load_win(gi)
                    for cc in range(4):
                        ch = (gi - 6) * 4 + cc
                        bk = bank()
                        for k in range(16):
                            wk = w.sub(k * 512 + cc * 128, k * 512 + (cc + 1) * 128)
                            hk = hT.sub(k * T, k * T + ncols)
                            mm(bk, bk.ap[:, 0:ncols], wk, wk.ap, hk, hk.ap, k == 0, k == 15)
                        dv = cT.sub(ch * T, ch * T + ncols)
                        tcopy("act", dv, dv.ap, bk, bk.ap[:, 0:ncols])
                for gi in (8, 9):
                    w = load_win(gi)
                    for cc in range(4):
                        ch = (gi - 8) * 4 + cc
                        bk = bank()
                        for k in range(16):
                            wk = w.sub(k * 512 + cc * 128, k * 512 + (cc + 1) * 128)
                            hk = hT.sub(k * T, k * T + ncols)
                            mm(bk, bk.ap[:, 0:ncols], wk, wk.ap, hk, hk.ap, k == 0, k == 15)
                        dv = gT.sub(ch * 516 + dst_col0, ch * 516 + dst_col0 + ncols)
                        cv = cT.sub(ch * T, ch * T + ncols)
                        tt("dve", dv, dv.ap, bk, bk.ap[:, 0:ncols], cv, cv.ap, ALU.mult)

            xh_v = xs.sub(0, D)
            memset("dve", xh_v, xh_v.ap, 0.0)
            dma(View(abuf, xh_v.lo, xh_v.hi, xh_v.ap[0:2, :], F32), DR(xh_d), "xs0")
            norm_transpose(xs, 1, hT, T, hbs, junk)
            conv_gate_cols(2, 514)

            for t in range(ntiles):
                r0 = t * T
                for b in range(NB):
                    dma(xs.sub(b * D, (b + 1) * D), DR(x_d[r0 + b * 128:r0 + (b + 1) * 128, :]), "xs%d" % b)
                dma(lng, DR(lng_d.partition_broadcast(128)), "lng")
                dma(lnb, DR(lnb_d.partition_broadcast(128)), "lnb")
                dma(msmall, DR(s_small[:, 0:5120]), "msmall")
                dma(bsb, DR(bs_d.partition_broadcast(128)), "c_bs")
                norm_transpose(xs, NB, hT, T, hbs, junk)

                for gi in (0, 1):
                    w = load_win(gi)
                    for cc in range(4):
                        ch = gi * 4 + cc
                        dv = uT.sub(ch * T, (ch + 1) * T)
                        proj_fm(w, cc, lambda bk, dv=dv: act(dv, dv.ap, bk, bk.ap, AF.Gelu_apprx_tanh))
                for gi in (2, 3):
                    w = load_win(gi)
                    for b in range(NB):
                        bk = bank()
                        for k in range(16):
                            hk = hT.sub(k * T + b * 128, k * T + (b + 1) * 128)
                            wk = w.sub(k * 512, (k + 1) * 512)
                            mm(bk, bk.ap, hk, hk.ap, wk, wk.ap, k == 0, k == 15)
                        dv = vtm.sub(b * 1024 + (gi - 2) * 512, b * 1024 + (gi - 1) * 512)
                        act(dv, dv.ap, bk, bk.ap, AF.Gelu_apprx_tanh)
                for b in range(NB):
                    vb = vtm.sub(b * 1024, (b + 1) * 1024)
                    S.op("dve", lambda e, o=bnst.ap[:, 0:6], i=vb.ap[:, 0:512]: e.bn_stats(out=o, in_=i), reads=[vb], writes=[bnst.sub(0, 6)])
                    S.op("dve", lambda e, o=bnst.ap[:, 6:12], i=vb.ap[:, 512:1024]: e.bn_stats(out=o, in_=i), reads=[vb], writes=[bnst.sub(6, 12)])
                    mv = bnst.sub(12, 14)
                    S.op("dve", lambda e, o=mv.ap, i=bnst.ap[:, 0:12]: e.bn_aggr(out=o, in_=i), reads=[bnst.sub(0, 12)], writes=[mv])
                    rs = bnst.sub(14, 15)
                    ts("dve", rs, rs.ap, mv, mv.ap[:, 1:2], EPS, ALU.add)
                    act(rs, rs.ap, rs, rs.ap, AF.Sqrt)
                    S.op("dve", lambda e, o=rs.ap: e.reciprocal(out=o, in_=o), reads=[rs], writes=[rs])
                    stt(vb, vb.ap, vb, vb.ap, mv.ap[:, 0:1], lng, lng.ap, ALU.subtract, ALU.mult, extra_r=[mv])
                    vl = vln.sub(b * 1024, (b + 1) * 1024)
                    stt(vl, vl.ap, vb, vb.ap, rs.ap, lnb, lnb.ap, ALU.mult, ALU.add, extra_r=[rs])
                for g in range(8):
                    bk = bank()
                    for b in range(NB):
                        vl = vln.sub(b * 1024 + g * 128, b * 1024 + (g + 1) * 128)
                        wg_ = wsT.sub(g * 128, (g + 1) * 128)
                        mm(bk, bk.ap[:, b * 128:(b + 1) * 128], vl, vl.ap, wg_, wg_.ap, True, True)
                    tmpv = tmpA
                    bsg = bsb.sub(g * 128, (g + 1) * 128)
                    tt("dve", tmpv, tmpv.ap.rearrange("p (b i) -> p b i", i=128), bk, bk.ap.rearrange("p (b i) -> p b i", i=128),
                       bsg, bsg.ap.unsqueeze(1).to_broadcast([128, NB, 128]), ALU.add)
                    dv = yT.sub(g * T, (g + 1) * T)
                    uv = uT.sub(g * T, (g + 1) * T)
                    tt("pool", dv, dv.ap, tmpv, tmpv.ap, uv, uv.ap, ALU.mult)

                gsrc = gT.ap.rearrange("p (c s) -> p c s", s=516)
                S.op("dve", lambda e, o=gsrc[:, :, 0:2], i=gsrc[:, :, 514:516]: e.tensor_copy(out=o, in_=i),
                     reads=[gT.span(0, 8 * 516, None)], writes=[gT.span(0, 8 * 516, None)])
                conv_gate_cols(T, 2)
                gsrc2 = gT.ap.rearrange("p (c s) -> p c s", s=516)
                S.op("pool", lambda e, o=gsrc2[:, :, 514:516], i=gsrc2[:, :, 512:514]: e.tensor_copy(out=o, in_=i),
                     reads=[gT.span(0, 8 * 516, None)], writes=[gT.span(0, 8 * 516, None)])
                for gi in (4, 5):
                    w = load_win(gi)
                    for cc in range(4):
                        ch = (gi - 4) * 4 + cc
                        gch = gT.sub(ch * 516, (ch + 1) * 516)
                        tv_ = tcv.sub((ch % 2) * T, (ch % 2 + 1) * T)
                        cwv = convw
                        ts("dve", tv_, tv_.ap, gch, gch.ap[:, 2:514], cwv.ap[:, 16 + ch:17 + ch], ALU.mult, extra_r=[cwv])
                        stt(tv_, tv_.ap, gch, gch.ap[:, 1:513], cwv.ap[:, 8 + ch:9 + ch], tv_, tv_.ap, ALU.mult, ALU.add, extra_r=[cwv])
                        stt(tv_, tv_.ap, gch, gch.ap[:, 0:512], cwv.ap[:, ch:ch + 1], tv_, tv_.ap, ALU.mult, ALU.add, extra_r=[cwv])
                        dv = yT.sub((8 + ch) * T, (9 + ch) * T)
                        proj_fm(w, cc, lambda bk, dv=dv, tv_=tv_: tt("dve", dv, dv.ap, bk, bk.ap, tv_, tv_.ap, ALU.mult))

                for gi in (10, 11):
                    w = load_win(gi)
                    for cc in range(4):
                        ch = (gi - 10) * 4 + cc
                        dv = qT.sub(ch * T, (ch + 1) * T)
                        proj_fm(w, cc, lambda bk, dv=dv: act(dv, dv.ap, bk, bk.ap, AF.Copy, scale=0.0625))
                for hd in range(4):
                    pv = pT.sub((hd % 2) * 2 * T, (hd % 2 + 1) * 2 * T)
                    for mc in range(2):
                        bk = bank()
                        for dc in range(2):
                            kk = kT.sub((2 * hd + dc) * 256 + mc * 128, (2 * hd + dc) * 256 + (mc + 1) * 128)
                            qq = qT.sub((2 * hd + dc) * T, (2 * hd + dc + 1) * T)
                            mm(bk, bk.ap, kk, kk.ap, qq, qq.ap, dc == 0, dc == 1)
                        pm = pv.sub(mc * T, (mc + 1) * T)
                        act(pm, pm.ap, bk, bk.ap, AF.Exp)
                    bD = bank()
                    for mc in range(2):
                        pm = pv.sub(mc * T, (mc + 1) * T)
                        mm(bD, bD.ap, onesb, onesb.ap, pm, pm.ap, mc == 0, mc == 1)
                    rv = rD.sub((hd % 2) * T, (hd % 2 + 1) * T)
                    S.op("dve", lambda e, o=rv.ap, i=bD.ap: e.reciprocal(out=o, in_=i), reads=[bD], writes=[rv])
                    for dc in range(2):
                        bO = bank()
                        for mc in range(2):
                            vv = vtok.sub(mc * 1024 + (2 * hd + dc) * 128, mc * 1024 + (2 * hd + dc + 1) * 128)
                            pm = pv.sub(mc * T, (mc + 1) * T)
                            mm(bO, bO.ap, vv, vv.ap, pm, pm.ap, mc == 0, mc == 1)
                        dv = yT.sub((16 + 2 * hd + dc) * T, (17 + 2 * hd + dc) * T)
                        tt("dve", dv, dv.ap, bO, bO.ap, rv, rv.ap, ALU.mult)

                for mg in range(4):
                    for n in range(3):
                        par = (mg * 3 + n) % 2
                        w = load_win(12 + n * 4 + mg)
                        for cc in range(4):
                            dv = sgt.sub((par * 4 + cc) * T, (par * 4 + cc + 1) * T)
                            proj_fm(w, cc, lambda bk, dv=dv: act(dv, dv.ap, bk, bk.ap, AF.Sigmoid))
                        wb = wbr_s[par]
                        dma(wb, DR(s_wbr[n * 4 + mg]), "wbr%d" % par)
                        for cc in range(4):
                            bk = bank()
                            for k in range(8):
                                wk = wb.sub(k * 512 + cc * 128, k * 512 + (cc + 1) * 128)
                                yk = yT.sub((n * 8 + k) * T, (n * 8 + k + 1) * T)
                                mm(bk, bk.ap, wk, wk.ap, yk, yk.ap, k == 0, k == 7)
                            sg_ = sgt.sub((par * 4 + cc) * T, (par * 4 + cc + 1) * T)
                            acc = mT.sub((mg * 4 + cc) * T, (mg * 4 + cc + 1) * T)
                            if n == 0:
                                tt("dve", acc, acc.ap, bk, bk.ap, sg_, sg_.ap, ALU.mult)
                            else:
                                tm = mtmp.sub((cc % 2) * T, (cc % 2 + 1) * T)
                                tt("dve", tm, tm.ap, bk, bk.ap, sg_, sg_.ap, ALU.mult)
                                tt("pool", acc, acc.ap, acc, acc.ap, tm, tm.ap, ALU.add)

                for cg in range(4):
                    w = win_s[wctr2[0] % 2]
                    sem = "win%d" % (wctr2[0] % 2)
                    wctr2[0] += 1
                    dma(w, DR(s_wout[cg]), sem)
                    for b in range(NB):
                        bk = bank()
                        for k in range(16):
                            mk = mT.sub(k * T + b * 128, k * T + (b + 1) * 128)
                            wk = w.sub(k * 512, (k + 1) * 512)
                            mm(bk, bk.ap, mk, mk.ap, wk, wk.ap, k == 0, k == 15)
                        xv = xs.sub(b * D + cg * 512, b * D + (cg + 1) * 512)
                        tt("dve", xv, xv.ap, bk, bk.ap, xv, xv.ap, ALU.add)

                if stop == "mixer":
                    dump("x1", xs, [128, NB * D])
                    dump("yT", yT, [128, 24 * T], BF16)
                    dump("mT", mT, [128, 16 * T], BF16)
                    dump("hT", hT, [128, 16 * T], BF16)
                checkpoint("mixer")
                norm_transpose(xs, NB, hT, T, hbs, junk)
                for cg in range(4):
                    w = win_s[wctr2[0] % 2]
                    sem = "win%d" % (wctr2[0] % 2)
                    wctr2[0] += 1
                    dma(w, DR(s_wq[cg]), sem)
                    for cc in range(4):
                        hp = cg * 4 + cc
                        dv = q2T.sub(hp * T, (hp + 1) * T)
                        proj_fm(w, cc, lambda bk, dv=dv: tcopy(rr_eng(["act", "dve"]), dv, dv.ap, bk, bk.ap))
                dma(keysT, DR(s_small[:, 5120:7168]), "keysT")
                for b in range(NB):
                    for q4 in range(4):
                        bk = bank()
                        for c4 in range(4):
                            hp = q4 * 4 + c4
                            qq = q2T.sub(hp * T + b * 128, hp * T + (b + 1) * 128)
                            kk = keysT.sub(hp * 128, (hp + 1) * 128)
                            mm(bk, bk.ap[:, c4 * 128:(c4 + 1) * 128], qq, qq.ap, kk, kk.ap, True, True)
                        dv = sc.sub(b * 2048 + q4 * 512, b * 2048 + (q4 + 1) * 512)
                        tcopy(rr_eng(["act", "dve"]), dv, dv.ap, bk, bk.ap)
                NR = NEXP // 512
                wdn = [0]
                wun = [0]
                wd_piece = {}
                wu_piece = {}

                def load_wd(p):
                    if p >= 2 * NR or p in wd_piece:
                        return
                    slot = p % 3
                    dma(wd_s[slot], DR(s_wdT[p]), "wd%d" % slot)
                    wd_piece[p] = wd_s[slot]

                def load_wu(p):
                    if p >= 4 * NR or p in wu_piece:
                        return
                    slot = p % 4
                    r_, cg_ = p // 4, p % 4
                    src = wup_d[r_ * 512:(r_ + 1) * 512, cg_ * 512:(cg_ + 1) * 512].rearrange("(cc j) c -> j cc c", j=128)
                    w_ = wu_s[slot]
                    dma(View(abuf, w_.lo, w_.hi, w_.ap.rearrange("p (cc c) -> p cc c", c=512), BF16), DR(src), "wu%d" % slot, eng="pool")
                    wu_piece[p] = wu_s[slot]

                dbk = [0]
                ubk = [0]

                def emit_down(r, cc):
                    half, c2 = cc // 2, cc % 2
                    p = r * 2 + half
                    if c2 == 0:
                        load_wd(p)
                        load_wd(p + 1)
                        load_wd(p + 2)
                    wd = wd_piece[p]
                    bk = bankx(2 + dbk[0] % 2)
                    dbk[0] += 1
                    for k in range(16):
                        wk = wd.sub(k * 256 + c2 * 128, k * 256 + (c2 + 1) * 128)
                        mm(bk, bk.ap, wk, wk.ap, hT_k(k), hT_k(k).ap, k == 0, k == 15)
                    dv = gd2[r % GDN].sub(cc * T, (cc + 1) * T)
                    act(dv, dv.ap, bk, bk.ap, AF.Gelu_apprx_tanh)

                cctr = [0]
                chain_slot = {}

                def emit_chainA(r, b):
                    sl = cctr[0] % NCH
                    cctr[0] += 1
                    chain_slot[(r, b)] = sl
                    Sg, Wg = Sg2[sl], Wg2[sl]
                    Eg = Sg
                    scb = sc.sub(b * 2048, (b + 1) * 2048)
                    s4 = scb.ap.rearrange("p (h t k) -> p h t k", h=8, t=2)
                    in0 = s4[:, :, 0, 4 * r:4 * r + 4].unsqueeze(3).to_broadcast([128, 8, 4, 128])
                    in1 = s4[:, :, 1, :].unsqueeze(2).to_broadcast([128, 8, 4, 128])
                    S.op("dve", lambda e, o=Sg.ap.rearrange("p (h i j) -> p h i j", h=8, i=4), a=in0, c=in1:
                         e.tensor_tensor(out=o, in0=a, in1=c, op=ALU.add), reads=[scb], writes=[Sg])
                    act(Eg, Eg.ap, Sg, Sg.ap, AF.Prelu, alpha=1.0e5)
                    act(Wg, Wg.ap, Eg, Eg.ap, AF.Exp)

                def emit_chainB(r, b):
                    pass

                def emit_gt(r, b):
                    Wg = Wg2[chain_slot[(r, b)]]
                    gbk = bankx(b % 2)
                    for cc in range(4):
                        for h in range(8):
                            wv = Wg.sub(h * 512 + cc * 128, h * 512 + (cc + 1) * 128)
                            dv = dgm.sub((b * 8 + h) * 128, (b * 8 + h + 1) * 128)
                            mm(gbk, gbk.ap[:, cc * 128:(cc + 1) * 128], wv, wv.ap, dv, dv.ap, h == 0, h == 7)
                    at = AT2[r % 2]
                    gdv = gd2[r % GDN]
                    oap = at.ap.rearrange("p (c s) -> p c s", s=T)[:, :, b * 128:(b + 1) * 128]
                    gap = gdv.ap.rearrange("p (c s) -> p c s", s=T)[:, :, b * 128:(b + 1) * 128]
                    tt("dve", at.span(b * 128, 3 * T + (b + 1) * 128, oap), oap, gbk, gbk.ap.rearrange("p (c s) -> p c s", s=128),
                       gdv.span(b * 128, 3 * T + (b + 1) * 128, gap), gap, ALU.mult)

                def emit_up(r, q):
                    cg = q
                    p = r * 4 + cg
                    load_wu(p)
                    load_wu(p + 1)
                    load_wu(p + 2)
                    load_wu(p + 3)
                    wu = wu_piece[p]
                    at = AT2[r % 2]
                    for b in range(NB):
                        bk = bankx(4 + ubk[0] % 4)
                        ubk[0] += 1
                        for cc in range(4):
                            av = at.sub(cc * T + b * 128, cc * T + (b + 1) * 128)
                            wv = wu.sub(cc * 512, (cc + 1) * 512)
                            mm(bk, bk.ap, av, av.ap, wv, wv.ap, cc == 0, cc == 3)
                        xv = xs.sub(b * D + cg * 512, b * D + (cg + 1) * 512)
                        tt("dve", xv, xv.ap, bk, bk.ap, xv, xv.ap, ALU.add)

                load_wd(0)
                load_wd(1)
                for r_ in range(GDN - 1):
                    for cc in range(4):
                        emit_down(r_, cc)
                AXX = mybir.AxisListType.X
                for b in range(NB):
                    svs = [sc.sub(b * 2048 + hp * 128, b * 2048 + (hp + 1) * 128) for hp in range(16)]
                    tps = [top.sub((b * 16 + hp) * 16, (b * 16 + hp + 1) * 16) for hp in range(16)]
                    wks = [cand8.sub(hp * 128, (hp + 1) * 128) for hp in range(16)]
                    for hp in range(16):
                        S.op("dve", lambda e, o=tps[hp].ap[:, 0:8], i=svs[hp].ap: e.max(out=o, in_=i), reads=[svs[hp]], writes=[tps[hp].sub(0, 8)])
                    for hp in range(16):
                        S.op("dve", lambda e, o=wks[hp].ap, r=tps[hp].ap[:, 0:8], i=svs[hp].ap: e.match_replace(out=o, in_to_replace=r, in_values=i, imm_value=NEG),
                             reads=[svs[hp], tps[hp].sub(0, 8)], writes=[wks[hp]])
                    for hp in range(16):
                        S.op("dve", lambda e, o=tps[hp].ap[:, 8:16], i=wks[hp].ap: e.max(out=o, in_=i), reads=[wks[hp]], writes=[tps[hp].sub(8, 16)])
                    tb = top.sub(b * 256, (b + 1) * 256)
                    scb_ = sc.sub(b * 2048, (b + 1) * 2048)
                    tt("dve", penb, penb.ap.rearrange("p (q k) -> p q k", k=128), scb_, scb_.ap.rearrange("p (q k) -> p q k", k=128),
                       tb, tb.ap.rearrange("p (q k) -> p q k", k=16)[:, :, 15:16].to_broadcast([128, 16, 128]), ALU.is_lt)
                    stt(scb_, scb_.ap, penb, penb.ap, -1.0e4, scb_, scb_.ap, ALU.mult, ALU.add)
                    t4 = tb.ap.rearrange("p (h t k) -> p h t k", h=8, t=2)
                    tt("pool", cand8, cand8.ap.rearrange("p (h a c) -> p h a c", h=8, a=16), tb, t4[:, :, 0, :].unsqueeze(3).to_broadcast([128, 8, 16, 16]),
                       tb, t4[:, :, 1, :].unsqueeze(2).to_broadcast([128, 8, 16, 16]), ALU.add)
                    cas = [cand8.sub(h * 256, (h + 1) * 256) for h in range(8)]
                    c24s = [c24.sub(h * 24, (h + 1) * 24) for h in range(8)]
                    for rnd in range(3):
                        for h in range(8):
                            S.op("dve", lambda e, o=c24s[h].ap[:, rnd * 8:rnd * 8 + 8], i=cas[h].ap: e.max(out=o, in_=i),
                                 reads=[cas[h]], writes=[c24s[h].sub(rnd * 8, rnd * 8 + 8)])
                        if rnd < 2:
                            for h in range(8):
                                S.op("dve", lambda e, o=cas[h].ap, r=c24s[h].ap[:, rnd * 8:rnd * 8 + 8]: e.match_replace(out=o, in_to_replace=r, in_values=o, imm_value=NEG),
                                     reads=[cas[h], c24s[h].sub(rnd * 8, rnd * 8 + 8)], writes=[cas[h]])
                    c3 = c24.ap.rearrange("p (h k) -> p h k", k=24)
                    thr = pst.sub(b * 8, b * 8 + 8)
                    lz = pst.sub(32 + b * 8, 32 + b * 8 + 8)
                    cbv = pst.sub(64 + b * 8, 64 + b * 8 + 8)
                    tt("dve", thr, thr.ap, c24, c3[:, :, 15], c24, c3[:, :, 16], ALU.add)
                    ts("dve", thr, thr.ap, thr, thr.ap, 0.5, ALU.mult)
                    d16 = candb.sub(0, 128)
                    tt("dve", d16, d16.ap.rearrange("p (h k) -> p h k", k=16), c24, c3[:, :, 0:16], c24, c3[:, :, 0:1].to_broadcast([128, 8, 16]), ALU.subtract)
                    act(d16, d16.ap, d16, d16.ap, AF.Exp)
                    S.op("dve", lambda e, o=lz.ap, i=d16.ap.rearrange("p (h k) -> p h k", k=16): e.tensor_reduce(out=o, in_=i, axis=AXX, op=ALU.add),
                         reads=[d16], writes=[lz])
                    act(lz, lz.ap, lz, lz.ap, AF.Ln)
                    tt("dve", cbv, cbv.ap, thr, thr.ap, c24, c3[:, :, 0], ALU.subtract)
                    tt("dve", cbv, cbv.ap, cbv, cbv.ap, lz, lz.ap, ALU.subtract)
                    ev = ecb.sub(b * 8, b * 8 + 8)
                    act(ev, ev.ap, cbv, cbv.ap, AF.Exp)
                    scb = sc.sub(b * 2048, (b + 1) * 2048)
                    s0ap = scb.ap.rearrange("p (h t k) -> p h t k", h=8, t=2)[:, :, 0, :]
                    tt("dve", scb.span(0, 2048, s0ap), s0ap, scb.span(0, 2048, s0ap), s0ap, thr, thr.ap.unsqueeze(2).to_broadcast([128, 8, 128]), ALU.subtract)
                    dv = dgm.sub(b * 1024, (b + 1) * 1024)
                    tt("pool", dv, dv.ap.rearrange("p (h s) -> p h s", s=128), identb, identb.ap.unsqueeze(1).to_broadcast([128, 8, 128]),
                       ev, ev.ap.unsqueeze(2).to_broadcast([128, 8, 128]), ALU.mult)
                if stop == "topk":
                    dump("sc", sc, [128, NB * 2048])
                    dump("top", top, [128, NB * 256])
                    dump("pst", pst, [128, NB * 32])
                    dump("ecb", ecb, [128, NB * 8])
                    dump("hT", hT, [128, 16 * T], BF16)
                checkpoint("topk")

                load_wu(0)
                load_wu(1)
                def blk(n):
                    return (n // NB, n % NB)

                NBLK = NR * NB
                for n0 in range(NCH):
                    emit_chainA(*blk(n0))
                for n in range(NBLK):
                    r, b = blk(n)
                    emit_gt(r, b)
                    if n + NCH < NBLK:
                        emit_chainA(*blk(n + NCH))
                    if r + GDN - 1 < NR and b % 2 == 0:
                        emit_down(r + GDN - 1, b)
                        emit_down(r + GDN - 1, b + 1)
                    if r >= 1:
                        emit_up(r - 1, b)
                for q in range(4):
                    emit_up(NR - 1, q)

                nfg = View(abuf, Sg2[0].lo, Sg2[0].lo + D * 4, A[:, Sg2[0].lo // 4:Sg2[0].lo // 4 + D], F32)
                dma(nfg, DR(gf_d.partition_broadcast(128)), "nfg")
                for b in range(NB):
                    xb = xs.sub(b * D, (b + 1) * D)
                    ss = stat.sub(16 + b, 17 + b)
                    rs = stat.sub(24 + b, 25 + b)
                    jk = Wg2[0].sub(0, D)
                    memset("pool", ss, ss.ap, 0.0)
                    act(jk, jk.ap, xb, xb.ap, AF.Square, extra_r=[ss], extra_w=[ss], accum_out=ss.ap)
                    ts("dve", rs, rs.ap, ss, ss.ap, 1.0 / D, ALU.mult, EPS, ALU.add)
                    act(rs, rs.ap, rs, rs.ap, AF.Sqrt)
                    S.op("dve", lambda e, o=rs.ap: e.reciprocal(out=o, in_=o), reads=[rs], writes=[rs])
                    osrc = (Sg2[1], Wg2[1])[b % 2]
                    ost = View(abuf, osrc.lo, osrc.lo + D * 4, A[:, osrc.lo // 4:osrc.lo // 4 + D], F32)
                    stt(ost, ost.ap, xb, xb.ap, rs.ap, nfg, nfg.ap, ALU.mult, ALU.mult, extra_r=[rs])
                    dma(View(ybuf, r0 + b * 128, r0 + (b + 1) * 128, y_d[r0 + b * 128:r0 + (b + 1) * 128, :]), ost, "ys%d" % (b % 2))

        except _Stop:
            pass
        S.dma_barrier("sp")
        S.emit(st)
        info = dict(nops=len(S.ops), nwait=S.nwait, nsem=S.nsem, arena=off[0])
    return nc, info


_CACHE = {}


def _prep_shared(inputs):
    f = lambda a: np.ascontiguousarray(np.asarray(a, dtype=np.float32))
    sh = {
        "norm_mix_g": f(inputs["norm_mix_g"]).reshape(D),
        "norm_mem_g": f(inputs["norm_mem_g"]).reshape(D),
        "w_in": f(inputs["w_in"]).reshape(D, DIN),
        "gmlp_w_s": f(inputs["gmlp_w_s"]).reshape(8, 128, 128),
        "gmlp_b_s": f(inputs["gmlp_b_s"]).reshape(8 * 128),
        "gmlp_ln_g": f(inputs["gmlp_ln_g"]).reshape(1024),
        "gmlp_ln_b": f(inputs["gmlp_ln_b"]).reshape(1024),
        "conv_w": f(inputs["conv_w"]).reshape(3, 1024),
        "w_kv": f(inputs["w_kv"]).reshape(D, 2048),
        "w_branch": f(inputs["w_branch"]).reshape(3, 1024, D),
        "w_out": f(inputs["w_out"]).reshape(D, D),
        "norm_ffn_g": f(inputs["norm_ffn_g"]).reshape(D),
        "peer_w_q": f(inputs["peer_w_q"]).reshape(D, 2048),
        "peer_keys": f(inputs["peer_keys"]).reshape(16, 128, 128),
        "peer_w_down": f(inputs["peer_w_down"]).reshape(NEXP, D),
        "peer_w_up": f(inputs["peer_w_up"]).reshape(NEXP, D),
        "norm_final_g": f(inputs["norm_final_g"]).reshape(D),
        "ident": np.eye(128, dtype=np.float32),
    }
    return sh


def kernel(**inputs):
    x = np.asarray(inputs["x"], dtype=np.float32)
    mem = np.asarray(inputs["mem"], dtype=np.float32)
    B, Sq, _ = x.shape
    ncores = 8
    per = (B * Sq) // ncores
    halves = Sq // per
    if "nc" not in _CACHE:
        _CACHE["nc"] = build(ntiles=per // T)[0]
    nc = _CACHE["nc"]
    sh = _prep_shared(inputs)
    in_maps = []
    for c in range(ncores):
        b, hf = c // halves, c % halves
        s0 = hf * per
        xh = np.zeros((2, D), np.float32)
        if s0 > 0:
            xh[:] = x[b, s0 - 2:s0]
        m = dict(sh)
        m["x"] = np.ascontiguousarray(x[b, s0:s0 + per])
        m["xh"] = xh
        m["mem"] = np.ascontiguousarray(mem[b])
        in_maps.append(m)
    res = run_bass_kernel_spmd(nc, in_maps, core_ids=list(range(ncores)))
    out = np.empty((B, Sq, D), np.float32)
    for c in range(ncores):
        b, hf = c // halves, c % halves
        out[b, hf * per:(hf + 1) * per] = res.results[c]["y"]
    return out
```

```python
import numpy as np
from contextlib import ExitStack
import concourse.bass as bass
import concourse.mybir as mybir
from concourse.bass_utils import run_bass_kernel_spmd

F32 = mybir.dt.float32
BF16 = mybir.dt.bfloat16
AF = mybir.ActivationFunctionType
ALU = mybir.AluOpType
DT_SIZE = {F32: 4, BF16: 2}

P = 128
D = 2048
DIN = 12288
T = 512
NB = 4
NEXP = 16384
NTOK_CORE = 4096
EPS = 1e-6
NEG = -1.0e30


class View:
    __slots__ = ("buf", "lo", "hi", "ap", "dtype", "dense")

    def __init__(self, buf, lo, hi, ap, dtype=None, dense=True):
        self.buf = buf
        self.lo = lo
        self.hi = hi
        self.ap = ap
        self.dtype = dtype
        self.dense = dense

    def sub(self, a, b):
        sz = DT_SIZE[self.dtype]
        return View(self.buf, self.lo + a * sz, self.lo + b * sz, self.ap[:, a:b], self.dtype, True)

    def span(self, a, b, ap):
        sz = DT_SIZE[self.dtype]
        return View(self.buf, self.lo + a * sz, self.lo + b * sz, ap, self.dtype, False)


class Buf:
    def __init__(self, name, handle=None, excl=False):
        self.name = name
        self.h = handle
        self.excl = excl
        self.last = []
        self.writers = []
        self.readers = {}


class Sched:
    def __init__(self, nc):
        self.nc = nc
        self.ops = []
        self.engobj = {"pe": nc.tensor, "act": nc.scalar, "dve": nc.vector,
                       "pool": nc.gpsimd, "sp": nc.sync}

    def _deps_for(self, reads, writes, opid, engkey):
        deps = set()
        ex = {}
        for v in reads:
            if v.buf.excl:
                ex.setdefault(id(v.buf), [v.buf, False])
        for v in writes:
            if v.buf.excl:
                ex.setdefault(id(v.buf), [v.buf, False])[1] = True
        for b, isw in ex.values():
            for (o, pw) in b.last:
                deps.add((o, "raw" if (pw and not isw) else "waw"))
            b.last = [(opid, isw)]
        reads = [v for v in reads if not v.buf.excl]
        writes = [v for v in writes if not v.buf.excl]
        for v in reads:
            for (lo, hi, o, _d) in v.buf.writers:
                if lo < v.hi and v.lo < hi:
                    deps.add((o, "raw"))
        for v in writes:
            b = v.buf
            for (lo, hi, o, _d) in b.writers:
                if lo < v.hi and v.lo < hi:
                    deps.add((o, "waw"))
            for (lo, hi, _k), o in b.readers.items():
                if lo < v.hi and v.lo < hi:
                    deps.add((o, "war"))
        for v in writes:
            b = v.buf
            if v.dense:
                b.writers = [w for w in b.writers if not (v.lo <= w[0] and w[1] <= v.hi)]
                b.readers = {k: o for k, o in b.readers.items() if not (v.lo <= k[0] and k[1] <= v.hi)}
            else:
                b.writers = [w for w in b.writers if not (w[0] == v.lo and w[1] == v.hi and not w[3])]
            b.writers.append((v.lo, v.hi, opid, v.dense))
        for v in reads:
            v.buf.readers[(v.lo, v.hi, engkey)] = opid
        return deps

    def op(self, eng, fn, reads=(), writes=(), dma_sem=None):
        opid = len(self.ops)
        engkey = eng if dma_sem is None else ("dma", dma_sem)
        deps = self._deps_for(reads, writes, opid, engkey)
        self.ops.append(dict(eng=eng, fn=fn, deps=deps, dma_sem=dma_sem, signal=False, barrier=False))
        return opid

    def dma_barrier(self, eng="sp"):
        self.ops.append(dict(eng=eng, fn=None, deps=set(), dma_sem=None, signal=False, barrier=True))

    def emit(self, stack):
        nc = self.nc
        ops = self.ops
        for o in ops:
            need = set()
            for (p, kind) in o["deps"]:
                po = ops[p]
                if po["dma_sem"] is None and po["eng"] == o["eng"] and o["dma_sem"] is None:
                    if o["eng"] == "pe":
                        continue
                need.add(p)
            latest = {}
            keep = set()
            for p in need:
                po = ops[p]
                if po["dma_sem"] is not None:
                    keep.add(p)
                elif po["eng"] not in latest or p > latest[po["eng"]]:
                    latest[po["eng"]] = p
            keep.update(latest.values())
            o["need"] = keep
            for p in keep:
                ops[p]["signal"] = True
        sems = {}
        for e in ("pe", "act", "dve", "pool"):
            sems[e] = stack.enter_context(nc.semaphore("s_" + e))
        for o in ops:
            if o["dma_sem"] is not None:
                k = ("dma", o["dma_sem"])
                if k not in sems:
                    sems[k] = stack.enter_context(nc.semaphore("d_" + str(o["dma_sem"])))
        count = {k: 0 for k in sems}
        waited = {e: {} for e in self.engobj}
        nwait = 0
        for o in ops:
            e = o["eng"]
            eo = self.engobj[e]
            if o["barrier"]:
                for sk, val in count.items():
                    if isinstance(sk, tuple) and val > 0 and waited[e].get(sk, 0) < val:
                        eo.wait_ge(sems[sk], val)
                        waited[e][sk] = val
                        nwait += 1
                continue
            tg = {}
            for p in o["need"]:
                po = ops[p]
                sk, val = po["sig"]
                if po["dma_sem"] is not None:
                    val = count[sk]
                if tg.get(sk, 0) < val:
                    tg[sk] = val
            for sk, val in tg.items():
                if waited[e].get(sk, 0) >= val:
                    continue
                eo.wait_ge(sems[sk], val)
                waited[e][sk] = val
                nwait += 1
            ins = o["fn"](eo)
            if o["dma_sem"] is not None:
                sk = ("dma", o["dma_sem"])
                count[sk] += 16
                ins.then_inc(sems[sk], 16)
                o["sig"] = (sk, count[sk])
            elif o["signal"]:
                count[e] += 1
                ins.then_inc(sems[e], 1)
                o["sig"] = (e, count[e])
            else:
                o["sig"] = None
        self.nwait = nwait
        self.nsem = len(sems)
        self.counts = count


class _Stop(Exception):
    pass


def build(ntiles=8, debug=False, stop=None):
    nc = bass.Bass("TRN2", target_bir_lowering=False)
    ntok = ntiles * T

    def din(name, shape):
        return nc.dram_tensor(name, list(shape), F32, kind="ExternalInput").ap()

    x_d = din("x", [ntok, D])
    xh_d = din("xh", [2, D])
    mem_d = din("mem", [256, D])
    g1_d = din("norm_mix_g", [D])
    gm_d = din("norm_mem_g", [D])
    win_d = din("w_in", [D, DIN])
    ws_d = din("gmlp_w_s", [8, 128, 128])
    bs_d = din("gmlp_b_s", [8 * 128])
    lng_d = din("gmlp_ln_g", [1024])
    lnb_d = din("gmlp_ln_b", [1024])
    cw_d = din("conv_w", [3, 1024])
    wkv_d = din("w_kv", [D, 2048])
    wbr_d = din("w_branch", [3, 1024, D])
    wout_d = din("w_out", [D, D])
    g2_d = din("norm_ffn_g", [D])
    wq_d = din("peer_w_q", [D, 2048])
    keys_d = din("peer_keys", [16, 128, 128])
    wdn_d = din("peer_w_down", [NEXP, D])
    wup_d = din("peer_w_up", [NEXP, D])
    gf_d = din("norm_final_g", [D])
    ident_d = din("ident", [128, 128])
    y_d = nc.dram_tensor("y", [ntok, D], F32, kind="ExternalOutput").ap()

    def dscr(name, shape):
        return nc.dram_tensor(name, list(shape), BF16, kind="Internal").ap()

    s_win = dscr("s_win", [24, 128, 16, 512])
    s_wbr = dscr("s_wbr", [12, 128, 8, 512])
    s_wout = dscr("s_wout", [4, 128, 16, 512])
    s_wq = dscr("s_wq", [4, 128, 16, 512])
    s_wdT = dscr("s_wdT", [64, 128, 16, 256])
    s_wup = dscr("s_wup", [128, 128, 4, 512])
    s_small = dscr("s_small", [128, 7168])

    dbg = {}

    st = ExitStack()
    with st:
        S = Sched(nc)
        ARENA_BYTES = 207 * 1024
        A = st.enter_context(nc.sbuf_tensor("arena", [128, ARENA_BYTES // 4], F32))
        abuf = Buf("arena", A)
        off = [0]

        def alloc(n, dt):
            nb = (n * DT_SIZE[dt] + 3) // 4 * 4
            lo = off[0]
            off[0] += nb
            assert off[0] <= ARENA_BYTES, ("arena overflow", off[0])
            ap = A[:, lo // 4:(lo + nb) // 4]
            if dt != F32:
                ap = ap.bitcast(dt)
            return View(abuf, lo, lo + nb, ap, dt)

        banks = []
        for i in range(8):
            t_ = st.enter_context(nc.psum_tensor("bank%d" % i, [128, 512], F32))
            banks.append(Buf("bank%d" % i, t_, excl=True))
        bctr = [0]

        def bank(dt=F32):
            i = bctr[0] % 8
            bctr[0] += 1
            ap = banks[i].h[:, :]
            if dt != F32:
                ap = ap.bitcast(dt)
            return View(banks[i], 0, 2048, ap, dt)

        rd_only = Buf("dram_ro")
        ybuf = Buf("y")
        scr_w = Buf("scratch_w")
        wctr = [0]

        def DR(ap):
            return View(rd_only, 0, 1, ap, None)

        def DW(ap):
            wctr[0] += 1
            return View(scr_w, wctr[0], wctr[0] + 1, ap, None)

        def dma(outv, inv, sem, eng="sp", **kw):
            S.op(eng, lambda e, o=outv.ap, i=inv.ap, kw=kw: e.dma_start(out=o, in_=i, **kw),
                 reads=[inv], writes=[outv], dma_sem=sem)

        def mm(outv, out_ap, lv, l_ap, rv, r_ap, start, stop):
            S.op("pe", lambda e, o=out_ap, l=l_ap, r=r_ap, a=start, b=stop:
                 e.matmul(o, lhsT=l, rhs=r, start=a, stop=b), reads=[lv, rv], writes=[outv])

        def tr(outv, out_ap, inv, in_ap, idv):
            S.op("pe", lambda e, o=out_ap, i=in_ap, d=idv.ap: e.transpose(out=o, in_=i, identity=d),
                 reads=[inv, idv], writes=[outv])

        def act(outv, out_ap, inv, in_ap, func, extra_r=(), extra_w=(), **kw):
            S.op("act", lambda e, o=out_ap, i=in_ap, f=func, kw=kw: e.activation(out=o, in_=i, func=f, **kw),
                 reads=[inv] + list(extra_r), writes=[outv] + list(extra_w))

        def tcopy(eng, outv, out_ap, inv, in_ap):
            if eng == "act":
                S.op("act", lambda e, o=out_ap, i=in_ap: e.copy(out=o, in_=i), reads=[inv], writes=[outv])
            else:
                S.op(eng, lambda e, o=out_ap, i=in_ap: e.tensor_copy(out=o, in_=i), reads=[inv], writes=[outv])

        def tt(eng, outv, out_ap, av, a_ap, bv, b_ap, op):
            S.op(eng, lambda e, o=out_ap, a=a_ap, b=b_ap, op=op: e.tensor_tensor(out=o, in0=a, in1=b, op=op),
                 reads=[av, bv], writes=[outv])

        def ts(eng, outv, out_ap, inv, in_ap, s1, op0, s2=None, op1=None, extra_r=()):
            def f(e, o=out_ap, i=in_ap, s1=s1, s2=s2, op0=op0, op1=op1):
                if op1 is None:
                    return e.tensor_scalar(out=o, in0=i, scalar1=s1, scalar2=None, op0=op0)
                return e.tensor_scalar(out=o, in0=i, scalar1=s1, scalar2=s2, op0=op0, op1=op1)
            S.op(eng, f, reads=[inv] + list(extra_r), writes=[outv])

        def stt(outv, out_ap, av, a_ap, scalar, bv, b_ap, op0, op1, extra_r=()):
            S.op("dve", lambda e, o=out_ap, a=a_ap, s=scalar, b=b_ap, op0=op0, op1=op1:
                 e.scalar_tensor_tensor(out=o, in0=a, scalar=s, in1=b, op0=op0, op1=op1),
                 reads=[av, bv] + list(extra_r), writes=[outv])

        def memset(eng, v, ap, val):
            S.op(eng, lambda e, a=ap, c=val: e.memset(a, c), writes=[v])

        rr = [0]

        def rr_eng(choices):
            rr[0] += 1
            return choices[rr[0] % len(choices)]

        xs = alloc(NB * D, F32)
        hT = alloc(16 * T, BF16)
        identf = alloc(128, F32)
        identb = alloc(128, BF16)
        onesb = alloc(128, BF16)
        convw = alloc(8 * 3, F32)
        g1f = alloc(16, F32)
        g2f = alloc(16, F32)
        gmf = alloc(16, F32)
        gT = alloc(8 * 516, BF16)
        stat = alloc(64, F32)
        PERSIST = off[0]

        def hT_k(k):
            return hT.sub(k * T, (k + 1) * T)

        dctr = [0]

        def dump(name, srcv, shape, dt=F32):
            o = nc.dram_tensor("dbg_" + name, list(shape), dt, kind="ExternalOutput").ap()
            dctr[0] += 1
            dma(View(ybuf, 10 ** 9 + dctr[0], 10 ** 9 + dctr[0] + 1, o), srcv, "dbg%d" % dctr[0])

        def checkpoint(name):
            if stop == name:
                raise _Stop()

        try:
            dma(identf, DR(ident_d), "c_id")
            tcopy("dve", identb, identb.ap, identf, identf.ap)
            memset("dve", onesb, onesb.ap, 1.0)
            for k_ in range(3):
                dma(convw.sub(k_ * 8, k_ * 8 + 8), DR(cw_d[k_].rearrange("(c p) -> p c", p=128)), "c_cw", allow_slow_non_contiguous=True)
            dma(g1f, DR(g1_d.rearrange("(k p) -> p k", p=128)), "c_g1", allow_slow_non_contiguous=True)
            dma(g2f, DR(g2_d.rearrange("(k p) -> p k", p=128)), "c_g2", allow_slow_non_contiguous=True)
            dma(gmf, DR(gm_d.rearrange("(k p) -> p k", p=128)), "c_gm", allow_slow_non_contiguous=True)

            def norm_transpose(src_v, nblk, dstT, dst_cols, tmp_hb, tmp_junk):
                for b in range(nblk):
                    xb = src_v.sub(b * D, (b + 1) * D)
                    ss = stat.sub(b, b + 1)
                    rs = stat.sub(8 + b, 9 + b)
                    hb = tmp_hb[b % len(tmp_hb)]
                    tmp_junk = hb
                    memset("pool", ss, ss.ap, 0.0)
                    act(tmp_junk, tmp_junk.ap, xb, xb.ap, AF.Square, extra_r=[ss], extra_w=[ss], accum_out=ss.ap)
                    ts("dve", rs, rs.ap, ss, ss.ap, 1.0 / D, ALU.mult, EPS, ALU.add)
                    act(rs, rs.ap, rs, rs.ap, AF.Sqrt)
                    S.op("dve", lambda e, o=rs.ap: e.reciprocal(out=o, in_=o), reads=[rs], writes=[rs])
                    act(hb, hb.ap, xb, xb.ap, AF.Copy, extra_r=[rs], scale=rs.ap)
                    for half in range(2):
                        bk = bank(BF16)
                        for kk in range(8):
                            k = half * 8 + kk
                            tr(bk, bk.ap[:, kk * 128:(kk + 1) * 128], hb, hb.ap[:, k * 128:(k + 1) * 128], identb)
                        a0 = half * 8 * dst_cols + b * 128
                        a1 = (half * 8 + 7) * dst_cols + (b + 1) * 128
                        dap = dstT.ap.rearrange("p (k s) -> p k s", s=dst_cols)[:, half * 8:half * 8 + 8, b * 128:(b + 1) * 128]
                        dv = dstT.span(a0, a1, dap)
                        tcopy(rr_eng(["act", "dve"]), dv, dap, bk, bk.ap.rearrange("p (k s) -> p k s", s=128))

            PRO = off[0]
            NSTG = 4
            stg = [alloc(4096, F32) for _ in range(NSTG)]
            stgb = [alloc(4096, BF16) for _ in range(NSTG)]
            pctr = [0]
            small = alloc(7168, BF16)
            kT = small.sub(0, 2048)
            vtok = small.sub(2048, 4096)
            wsT = small.sub(4096, 5120)
            keysT = small.sub(5120, 7168)

            pieces = []

            def run_pieces():
                n = len(pieces)
                base = pctr[0]
                for j in range(min(NSTG - 1, n)):
                    pieces[j][0]((base + j) % NSTG)
                for i_ in range(n):
                    j = i_ + NSTG - 1
                    if j < n:
                        pieces[j][0]((base + j) % NSTG)
                    pieces[i_][1]((base + i_) % NSTG)
                pctr[0] += n
                del pieces[:]

            def cast_rows(w_ap, nrows, ncols, gain, store_fn):
                cp = min(ncols, 4096)
                for k in range(nrows // 128):
                    for c0 in range(0, ncols, cp):
                        def ld(i, k=k, c0=c0):
                            sv = stg[i].sub(0, cp)
                            dma(sv, DR(w_ap[k * 128:(k + 1) * 128, c0:c0 + cp]), "stg%d" % i)

                        def wk(i, k=k, c0=c0):
                            sv = stg[i].sub(0, cp)
                            bv = stgb[i].sub(0, cp)
                            eng = rr_eng(["dve", "act", "act"])
                            if gain is None:
                                tcopy(eng, bv, bv.ap, sv, sv.ap)
                            elif eng == "act":
                                act(bv, bv.ap, sv, sv.ap, AF.Copy, extra_r=[gain], scale=gain.ap[:, k:k + 1])
                            else:
                                ts(eng, bv, bv.ap, sv, sv.ap, gain.ap[:, k:k + 1], ALU.mult, extra_r=[gain])
                            store_fn(k, c0, cp, bv, "stgb%d" % i)
                        pieces.append((ld, wk))

            def st_win(k, c0, cp, bv, sem):
                g0 = c0 // 512
                ng = cp // 512
                dma(DW(s_win[g0:g0 + ng, :, k, :].rearrange("g p c -> p g c")),
                    View(bv.buf, bv.lo, bv.hi, bv.ap.rearrange("p (g c) -> p g c", c=512)), sem)
            cast_rows(win_d, D, DIN, g1f, st_win)

            for n in range(3):
                def st_wbr(k, c0, cp, bv, sem, n=n):
                    dma(DW(s_wbr[n * 4:(n + 1) * 4, :, k, :].rearrange("g p c -> p g c")),
                        View(bv.buf, bv.lo, bv.hi, bv.ap.rearrange("p (g c) -> p g c", c=512)), sem)
                cast_rows(wbr_d[n], 1024, D, None, st_wbr)

            def st_wout(k, c0, cp, bv, sem):
                dma(DW(s_wout[:, :, k, :].rearrange("g p c -> p g c")),
                    View(bv.buf, bv.lo, bv.hi, bv.ap.rearrange("p (g c) -> p g c", c=512)), sem)
            cast_rows(wout_d, D, D, None, st_wout)

            def st_wq(k, c0, cp, bv, sem):
                dma(DW(s_wq[:, :, k, :].rearrange("g p c -> p g c")),
                    View(bv.buf, bv.lo, bv.hi, bv.ap.rearrange("p (g c) -> p g c", c=512)), sem)
            cast_rows(wq_d, D, 2048, g2f, st_wq)

            def st_wup(k, c0, cp, bv, sem):
                r, cc = k // 4, k % 4
                dma(DW(s_wup[r * 4:(r + 1) * 4, :, cc, :].rearrange("g p c -> p g c")),
                    View(bv.buf, bv.lo, bv.hi, bv.ap.rearrange("p (g c) -> p g c", c=512)), sem)
            run_pieces()

            PRO2 = off[0]
            stgT = [alloc(16 * 512, BF16) for _ in range(2)]
            for eb in range(NEXP // 128):
                def ld(i, eb=eb):
                    sv = stg[i].sub(0, D)
                    dma(sv, DR(wdn_d[eb * 128:(eb + 1) * 128, :]), "stg%d" % i)

                def wk(i, eb=eb):
                    r, cc = eb // 4, eb % 4
                    sv = stg[i].sub(0, D)
                    tv = stgT[r % 2]
                    for q in range(4):
                        bk = bank()
                        for kk in range(4):
                            k = q * 4 + kk
                            tr(bk, bk.ap[:, kk * 128:(kk + 1) * 128], sv, sv.ap[:, k * 128:(k + 1) * 128], identf)
                        eng = rr_eng(["dve", "act"])
                        for kk in range(4):
                            k = q * 4 + kk
                            a0 = k * 512 + cc * 128
                            dv = tv.sub(a0, a0 + 128)
                            src = View(bk.buf, kk * 512, kk * 512 + 512, bk.ap[:, kk * 128:(kk + 1) * 128], F32)
                            if eng == "act":
                                act(dv, dv.ap, src, src.ap, AF.Copy, extra_r=[g2f], scale=g2f.ap[:, k:k + 1])
                            else:
                                ts("dve", dv, dv.ap, src, src.ap, g2f.ap[:, k:k + 1], ALU.mult, extra_r=[g2f])
                    if cc == 3:
                        for half in range(2):
                            sap = tv.ap.rearrange("p (k e) -> p k e", e=512)[:, :, half * 256:(half + 1) * 256]
                            dma(DW(s_wdT[r * 2 + half]), tv.span(0, 16 * 512, sap), "stgT%d" % (r % 2))
                pieces.append((ld, wk))
            run_pieces()

            ktmp = stg[0].sub(0, 128)
            for hp in range(16):
                kv_ = stg[hp % 2].sub(0, 128)
                dma(kv_, DR(keys_d[hp]), "stg%d" % (hp % 2))
                bk = bank()
                tr(bk, bk.ap[:, 0:128], kv_, kv_.ap, identf)
                dv = keysT.sub(hp * 128, (hp + 1) * 128)
                tcopy("dve", dv, dv.ap, View(bk.buf, 0, 512, bk.ap[:, 0:128], F32), bk.ap[:, 0:128])
            for g in range(8):
                wv_ = stg[g % 2].sub(0, 128)
                dma(wv_, DR(ws_d[g]), "stg%d" % (g % 2))
                bk = bank()
                tr(bk, bk.ap[:, 0:128], wv_, wv_.ap, identf)
                dv = wsT.sub(g * 128, (g + 1) * 128)
                tcopy("dve", dv, dv.ap, View(bk.buf, 0, 512, bk.ap[:, 0:128], F32), bk.ap[:, 0:128])
                memset("dve", dv, dv.ap[64:128, 0:64], 0.0)

            off[0] = PRO2
            memx = alloc(2 * D, F32)
            memT = alloc(16 * 256, BF16)
            hb_m = [alloc(D, BF16)]
            junk_m = None
            wkb = [alloc(1024, BF16) for _ in range(2)]
            for b in range(2):
                dma(memx.sub(b * D, (b + 1) * D), DR(mem_d[b * 128:(b + 1) * 128, :]), "memx")
            norm_transpose(memx, 2, memT, 256, hb_m, junk_m)
            for part in range(2):
                accs = [bank() for _ in range(8 if part == 0 else 4)]
                for k in range(16):
                    i = pctr[0] % NSTG
                    pctr[0] += 1
                    sv = stg[i].sub(0, 1024)
                    dma(sv, DR(wkv_d[k * 128:(k + 1) * 128, part * 1024:(part + 1) * 1024]), "stg%d" % i)
                    wb = wkb[k % 2]
                    ts("dve", wb, wb.ap, sv, sv.ap, gmf.ap[:, k:k + 1], ALU.mult, extra_r=[gmf])
                    mk = memT.sub(k * 256, (k + 1) * 256)
                    if part == 0:
                        for c in range(8):
                            mm(accs[c], accs[c].ap[:, 0:256], wb, wb.ap[:, c * 128:(c + 1) * 128], mk, mk.ap, k == 0, k == 15)
                    else:
                        for mc in range(2):
                            for cg in range(2):
                                a = accs[mc * 2 + cg]
                                mm(a, a.ap, mk, mk.ap[:, mc * 128:(mc + 1) * 128], wb, wb.ap[:, cg * 512:(cg + 1) * 512], k == 0, k == 15)
                if part == 0:
                    for c in range(8):
                        dv = kT.sub(c * 256, (c + 1) * 256)
                        tcopy(rr_eng(["act", "dve"]), dv, dv.ap, accs[c], accs[c].ap[:, 0:256])
                else:
                    for mc in range(2):
                        for cg in range(2):
                            dv = vtok.sub(mc * 1024 + cg * 512, mc * 1024 + (cg + 1) * 512)
                            tcopy(rr_eng(["act", "dve"]), dv, dv.ap, accs[mc * 2 + cg], accs[mc * 2 + cg].ap)

            dma(DW(s_small), small, "small_st")
            S.dma_barrier("sp")
            if stop == "pro":
                dump("kT", kT, [128, 8 * 256], BF16)
                dump("vtok", vtok, [128, 2048], BF16)
                dump("keysT", keysT, [128, 2048], BF16)
                dump("wsT", wsT, [128, 1024], BF16)
                dump("memT", memT, [128, 16 * 256], BF16)
                dump("s_win0", DR(s_win[0]), [128, 16, 512], BF16)
                dump("s_win23", DR(s_win[23]), [128, 16, 512], BF16)
                dump("s_wbr5", DR(s_wbr[5]), [128, 8, 512], BF16)
                dump("s_wout1", DR(s_wout[1]), [128, 16, 512], BF16)
                dump("s_wq2", DR(s_wq[2]), [128, 16, 512], BF16)
                dump("s_wdT3", DR(s_wdT[3]), [128, 16, 256], BF16)
            checkpoint("pro")
            off[0] = PRO

            MIX = off[0]
            win_s = [alloc(16 * 512, BF16) for _ in range(2)]
            wctr2 = [0]
            yT = alloc(24 * T, BF16)
            sgt = alloc(8 * T, BF16)
            wbr_s = [alloc(8 * 512, BF16) for _ in range(2)]
            hbs = [alloc(D, BF16) for _ in range(2)]
            junk = None
            msmall = alloc(5120, BF16)
            kT = msmall.sub(0, 2048)
            vtok = msmall.sub(2048, 4096)
            wsT = msmall.sub(4096, 5120)
            bsb = alloc(8 * 128, F32)
            SCR = off[0]
            uT = alloc(8 * T, BF16)
            vtm = alloc(NB * 1024, F32)
            vln = alloc(NB * 1024, BF16)
            lng = alloc(1024, F32)
            lnb = alloc(1024, F32)
            bnst = alloc(16, F32)
            tmpA = alloc(T, F32)
            SCR_END = off[0]
            off[0] = SCR
            cT = alloc(8 * T, BF16)
            tcv = alloc(2 * T, F32)
            off[0] = SCR
            qT = alloc(8 * T, BF16)
            pT = alloc(2 * 2 * T, BF16)
            rD = alloc(2 * T, F32)
            off[0] = SCR
            mT = alloc(16 * T, BF16)
            mtmp = alloc(2 * T, F32)
            assert off[0] <= SCR_END
            off[0] = SCR_END
            MIX_END = off[0]

            off[0] = MIX
            sc = alloc(NB * 16 * 128, F32)
            c24 = alloc(8 * 24, F32)
            pst = alloc(NB * 8 * 4, F32)
            ecb = alloc(NB * 8, F32)
            dgm = alloc(NB * 8 * 128, BF16)
            wd_s = [alloc(16 * 256, BF16) for _ in range(3)]
            wu_s = [alloc(4 * 512, BF16) for _ in range(4)]
            GA = off[0]
            GDN = 6
            gd2 = [alloc(4 * T, BF16) for _ in range(GDN)]
            ATB = off[0]
            AT2 = [alloc(4 * T, BF16) for _ in range(2)]
            GA_END = off[0]
            off[0] = ATB
            top = alloc(NB * 16 * 16, F32)
            candb = alloc(2 * 256, F32)
            assert off[0] <= GA_END
            off[0] = GA_END
            GRID = off[0]
            NCH = 2
            Sg2 = [alloc(4096, BF16) for _ in range(NCH)]
            Wg2 = [alloc(4096, BF16) for _ in range(NCH)]
            GRID_END = off[0]
            off[0] = GRID
            q2T = alloc(16 * T, BF16)
            keysT = alloc(16 * 128, BF16)
            cand8 = alloc(8 * 256, F32)
            penb = alloc(16 * 128, BF16)
            assert off[0] <= GRID_END
            PEER_END = GRID_END
            total_bytes = max(MIX_END, PEER_END)
            assert total_bytes <= ARENA_BYTES, total_bytes

            def bankx(i, dt=F32):
                ap = banks[i].h[:, :]
                if dt != F32:
                    ap = ap.bitcast(dt)
                return View(banks[i], 0, 2048, ap, dt)

            def load_win(g):
                w = win_s[wctr2[0] % 2]
                sem = "win%d" % (wctr2[0] % 2)
                wctr2[0] += 1
                dma(w, DR(s_win[g]), sem)
                return w

            def proj_fm(w, cc, evac):
                bk = bank()
                for k in range(16):
                    wk = w.sub(k * 512 + cc * 128, k * 512 + (cc + 1) * 128)
                    mm(bk, bk.ap, wk, wk.ap, hT_k(k), hT_k(k).ap, k == 0, k == 15)
                evac(bk)

            def conv_gate_cols(ncols, dst_col0):
                for gi in (6, 7):
                    w = load_win(gi)
                    for cc in range(4):
                        ch = (gi - 6) * 4 + cc
                        bk = bank()
                        for k in range(16):
                            wk = w.sub(k * 512 + cc * 128, k * 512 + (cc + 1) * 128)
                            hk = hT.sub(k * T, k * T + ncols)
                            mm(bk, bk.ap[:, 0:ncols], wk, wk.ap, hk, hk.ap, k == 0, k == 15)
                        dv = cT.sub(ch * T, ch * T + ncols)
                        tcopy("act", dv, dv.ap, bk, bk.ap[:, 0:ncols])
                for gi in (8, 9):
                    w = load_win(gi)
                    for cc in range(4):
                        ch = (gi - 8) * 4 + cc
                        bk = bank()
                        for k in range(16):
                            wk = w.sub(k * 512 + cc * 128, k * 512 + (cc + 1) * 128)
                            hk = hT.sub(k * T, k * T + ncols)
                            mm(bk, bk.ap[:, 0:ncols], wk, wk.ap, hk, hk.ap, k == 0, k == 15)
                        dv = gT.sub(ch * 516 + dst_col0, ch * 516 + dst_col0 + ncols)
                        cv = cT.sub(ch * T, ch * T + ncols)
                        tt("dve", dv, dv.ap, bk, bk.ap[:, 0:ncols], cv, cv.ap, ALU.mult)

            xh_v = xs.sub(0, D)
            memset("dve", xh_v, xh_v.ap, 0.0)
            dma(View(abuf, xh_v.lo, xh_v.hi, xh_v.ap[0:2, :], F32), DR(xh_d), "xs0")
            norm_transpose(xs, 1, hT, T, hbs, junk)
            conv_gate_cols(2, 514)

            for t in range(ntiles):
                r0 = t * T
                if t == 1:
                    S.dma_barrier("sp")
                for b in range(NB):
                    dma(xs.sub(b * D, (b + 1) * D), DR(x_d[r0 + b * 128:r0 + (b + 1) * 128, :]), "xs%d" % b)
                dma(lng, DR(lng_d.partition_broadcast(128)), "lng")
                dma(lnb, DR(lnb_d.partition_broadcast(128)), "lnb")
                dma(msmall, DR(s_small[:, 0:5120]), "msmall")
                dma(bsb, DR(bs_d.partition_broadcast(128)), "c_bs")
                norm_transpose(xs, NB, hT, T, hbs, junk)

                for gi in (0, 1):
                    w = load_win(gi)
                    for cc in range(4):
                        ch = gi * 4 + cc
                        dv = uT.sub(ch * T, (ch + 1) * T)
                        proj_fm(w, cc, lambda bk, dv=dv: act(dv, dv.ap, bk, bk.ap, AF.Gelu_apprx_tanh))
                for gi in (2, 3):
                    w = load_win(gi)
                    for b in range(NB):
                        bk = bank()
                        for k in range(16):
                            hk = hT.sub(k * T + b * 128, k * T + (b + 1) * 128)
                            wk = w.sub(k * 512, (k + 1) * 512)
                            mm(bk, bk.ap, hk, hk.ap, wk, wk.ap, k == 0, k == 15)
                        dv = vtm.sub(b * 1024 + (gi - 2) * 512, b * 1024 + (gi - 1) * 512)
                        act(dv, dv.ap, bk, bk.ap, AF.Gelu_apprx_tanh)
                for b in range(NB):
                    vb = vtm.sub(b * 1024, (b + 1) * 1024)
                    S.op("dve", lambda e, o=bnst.ap[:, 0:6], i=vb.ap[:, 0:512]: e.bn_stats(out=o, in_=i), reads=[vb], writes=[bnst.sub(0, 6)])
                    S.op("dve", lambda e, o=bnst.ap[:, 6:12], i=vb.ap[:, 512:1024]: e.bn_stats(out=o, in_=i), reads=[vb], writes=[bnst.sub(6, 12)])
                    mv = bnst.sub(12, 14)
                    S.op("dve", lambda e, o=mv.ap, i=bnst.ap[:, 0:12]: e.bn_aggr(out=o, in_=i), reads=[bnst.sub(0, 12)], writes=[mv])
                    rs = bnst.sub(14, 15)
                    ts("dve", rs, rs.ap, mv, mv.ap[:, 1:2], EPS, ALU.add)
                    act(rs, rs.ap, rs, rs.ap, AF.Sqrt)
                    S.op("dve", lambda e, o=rs.ap: e.reciprocal(out=o, in_=o), reads=[rs], writes=[rs])
                    stt(vb, vb.ap, vb, vb.ap, mv.ap[:, 0:1], lng, lng.ap, ALU.subtract, ALU.mult, extra_r=[mv])
                    vl = vln.sub(b * 1024, (b + 1) * 1024)
                    stt(vl, vl.ap, vb, vb.ap, rs.ap, lnb, lnb.ap, ALU.mult, ALU.add, extra_r=[rs])
                for g in range(8):
                    bk = bank()
                    for b in range(NB):
                        vl = vln.sub(b * 1024 + g * 128, b * 1024 + (g + 1) * 128)
                        wg_ = wsT.sub(g * 128, (g + 1) * 128)
                        mm(bk, bk.ap[:, b * 128:(b + 1) * 128], vl, vl.ap, wg_, wg_.ap, True, True)
                    tmpv = tmpA
                    bsg = bsb.sub(g * 128, (g + 1) * 128)
                    tt("dve", tmpv, tmpv.ap.rearrange("p (b i) -> p b i", i=128), bk, bk.ap.rearrange("p (b i) -> p b i", i=128),
                       bsg, bsg.ap.unsqueeze(1).to_broadcast([128, NB, 128]), ALU.add)
                    dv = yT.sub(g * T, (g + 1) * T)
                    uv = uT.sub(g * T, (g + 1) * T)
                    tt("pool", dv, dv.ap, tmpv, tmpv.ap, uv, uv.ap, ALU.mult)

                gsrc = gT.ap.rearrange("p (c s) -> p c s", s=516)
                S.op("dve", lambda e, o=gsrc[:, :, 0:2], i=gsrc[:, :, 514:516]: e.tensor_copy(out=o, in_=i),
                     reads=[gT.span(0, 8 * 516, None)], writes=[gT.span(0, 8 * 516, None)])
                conv_gate_cols(T, 2)
                gsrc2 = gT.ap.rearrange("p (c s) -> p c s", s=516)
                S.op("pool", lambda e, o=gsrc2[:, :, 514:516], i=gsrc2[:, :, 512:514]: e.tensor_copy(out=o, in_=i),
                     reads=[gT.span(0, 8 * 516, None)], writes=[gT.span(0, 8 * 516, None)])
                for gi in (4, 5):
                    w = load_win(gi)
                    for cc in range(4):
                        ch = (gi - 4) * 4 + cc
                        gch = gT.sub(ch * 516, (ch + 1) * 516)
                        tv_ = tcv.sub((ch % 2) * T, (ch % 2 + 1) * T)
                        cwv = convw
                        ts("dve", tv_, tv_.ap, gch, gch.ap[:, 2:514], cwv.ap[:, 16 + ch:17 + ch], ALU.mult, extra_r=[cwv])
                        stt(tv_, tv_.ap, gch, gch.ap[:, 1:513], cwv.ap[:, 8 + ch:9 + ch], tv_, tv_.ap, ALU.mult, ALU.add, extra_r=[cwv])
                        stt(tv_, tv_.ap, gch, gch.ap[:, 0:512], cwv.ap[:, ch:ch + 1], tv_, tv_.ap, ALU.mult, ALU.add, extra_r=[cwv])
                        dv = yT.sub((8 + ch) * T, (9 + ch) * T)
                        proj_fm(w, cc, lambda bk, dv=dv, tv_=tv_: tt("dve", dv, dv.ap, bk, bk.ap, tv_, tv_.ap, ALU.mult))

                for gi in (10, 11):
                    w = load_win(gi)
                    for cc in range(4):
                        ch = (gi - 10) * 4 + cc
                        dv = qT.sub(ch * T, (ch + 1) * T)
                        proj_fm(w, cc, lambda bk, dv=dv: act(dv, dv.ap, bk, bk.ap, AF.Copy, scale=0.0625))
                for hd in range(4):
                    pv = pT.sub((hd % 2) * 2 * T, (hd % 2 + 1) * 2 * T)
                    for mc in range(2):
                        bk = bank()
                        for dc in range(2):
                            kk = kT.sub((2 * hd + dc) * 256 + mc * 128, (2 * hd + dc) * 256 + (mc + 1) * 128)
                            qq = qT.sub((2 * hd + dc) * T, (2 * hd + dc + 1) * T)
                            mm(bk, bk.ap, kk, kk.ap, qq, qq.ap, dc == 0, dc == 1)
                        pm = pv.sub(mc * T, (mc + 1) * T)
                        act(pm, pm.ap, bk, bk.ap, AF.Exp)
                    bD = bank()
                    for mc in range(2):
                        pm = pv.sub(mc * T, (mc + 1) * T)
                        mm(bD, bD.ap, onesb, onesb.ap, pm, pm.ap, mc == 0, mc == 1)
                    rv = rD.sub((hd % 2) * T, (hd % 2 + 1) * T)
                    S.op("dve", lambda e, o=rv.ap, i=bD.ap: e.reciprocal(out=o, in_=i), reads=[bD], writes=[rv])
                    for dc in range(2):
                        bO = bank()
                        for mc in range(2):
                            vv = vtok.sub(mc * 1024 + (2 * hd + dc) * 128, mc * 1024 + (2 * hd + dc + 1) * 128)
                            pm = pv.sub(mc * T, (mc + 1) * T)
                            mm(bO, bO.ap, vv, vv.ap, pm, pm.ap, mc == 0, mc == 1)
                        dv = yT.sub((16 + 2 * hd + dc) * T, (17 + 2 * hd + dc) * T)
                        tt("dve", dv, dv.ap, bO, bO.ap, rv, rv.ap, ALU.mult)

                for mg in range(4):
                    for n in range(3):
                        par = (mg * 3 + n) % 2
                        w = load_win(12 + n * 4 + mg)
                        for cc in range(4):
                            dv = sgt.sub((par * 4 + cc) * T, (par * 4 + cc + 1) * T)
                            proj_fm(w, cc, lambda bk, dv=dv: act(dv, dv.ap, bk, bk.ap, AF.Sigmoid))
                        wb = wbr_s[par]
                        dma(wb, DR(s_wbr[n * 4 + mg]), "wbr%d" % par)
                        for cc in range(4):
                            bk = bank()
                            for k in range(8):
                                wk = wb.sub(k * 512 + cc * 128, k * 512 + (cc + 1) * 128)
                                yk = yT.sub((n * 8 + k) * T, (n * 8 + k + 1) * T)
                                mm(bk, bk.ap, wk, wk.ap, yk, yk.ap, k == 0, k == 7)
                            sg_ = sgt.sub((par * 4 + cc) * T, (par * 4 + cc + 1) * T)
                            acc = mT.sub((mg * 4 + cc) * T, (mg * 4 + cc + 1) * T)
                            if n == 0:
                                tt("dve", acc, acc.ap, bk, bk.ap, sg_, sg_.ap, ALU.mult)
                            else:
                                tm = mtmp.sub((cc % 2) * T, (cc % 2 + 1) * T)
                                tt("dve", tm, tm.ap, bk, bk.ap, sg_, sg_.ap, ALU.mult)
                                tt("pool", acc, acc.ap, acc, acc.ap, tm, tm.ap, ALU.add)

                for cg in range(4):
                    w = win_s[wctr2[0] % 2]
                    sem = "win%d" % (wctr2[0] % 2)
                    wctr2[0] += 1
                    dma(w, DR(s_wout[cg]), sem)
                    for b in range(NB):
                        bk = bank()
                        for k in range(16):
                            mk = mT.sub(k * T + b * 128, k * T + (b + 1) * 128)
                            wk = w.sub(k * 512, (k + 1) * 512)
                            mm(bk, bk.ap, mk, mk.ap, wk, wk.ap, k == 0, k == 15)
                        xv = xs.sub(b * D + cg * 512, b * D + (cg + 1) * 512)
                        tt("dve", xv, xv.ap, bk, bk.ap, xv, xv.ap, ALU.add)

                if stop == "mixer":
                    dump("x1", xs, [128, NB * D])
                    dump("yT", yT, [128, 24 * T], BF16)
                    dump("mT", mT, [128, 16 * T], BF16)
                    dump("hT", hT, [128, 16 * T], BF16)
                checkpoint("mixer")
                norm_transpose(xs, NB, hT, T, hbs, junk)
                for cg in range(4):
                    w = win_s[wctr2[0] % 2]
                    sem = "win%d" % (wctr2[0] % 2)
                    wctr2[0] += 1
                    dma(w, DR(s_wq[cg]), sem)
                    for cc in range(4):
                        hp = cg * 4 + cc
                        dv = q2T.sub(hp * T, (hp + 1) * T)
                        proj_fm(w, cc, lambda bk, dv=dv: tcopy(rr_eng(["act", "dve"]), dv, dv.ap, bk, bk.ap))
                dma(keysT, DR(s_small[:, 5120:7168]), "keysT")
                for b in range(NB):
                    for q4 in range(4):
                        bk = bank()
                        for c4 in range(4):
                            hp = q4 * 4 + c4
                            qq = q2T.sub(hp * T + b * 128, hp * T + (b + 1) * 128)
                            kk = keysT.sub(hp * 128, (hp + 1) * 128)
                            mm(bk, bk.ap[:, c4 * 128:(c4 + 1) * 128], qq, qq.ap, kk, kk.ap, True, True)
                        dv = sc.sub(b * 2048 + q4 * 512, b * 2048 + (q4 + 1) * 512)
                        tcopy(rr_eng(["act", "dve"]), dv, dv.ap, bk, bk.ap)
                NR = NEXP // 512
                wdn = [0]
                wun = [0]
                wd_piece = {}
                wu_piece = {}

                def load_wd(p):
                    if p >= 2 * NR or p in wd_piece:
                        return
                    slot = p % 3
                    dma(wd_s[slot], DR(s_wdT[p]), "wd%d" % slot)
                    wd_piece[p] = wd_s[slot]

                def load_wu(p):
                    if p >= 4 * NR or p in wu_piece:
                        return
                    slot = p % 4
                    r_, cg_ = p // 4, p % 4
                    src = wup_d[r_ * 512:(r_ + 1) * 512, cg_ * 512:(cg_ + 1) * 512].rearrange("(cc j) c -> j cc c", j=128)
                    w_ = wu_s[slot]
                    if t == 0:
                        dma(View(abuf, w_.lo, w_.hi, w_.ap.rearrange("p (cc c) -> p cc c", c=512), BF16), DR(src), "wu%d" % slot, eng="pool")
                        dma(DW(s_wup[p]), w_, "wus%d" % slot)
                    else:
                        dma(w_, DR(s_wup[p]), "wu%d" % slot)
                    wu_piece[p] = wu_s[slot]

                dbk = [0]
                ubk = [0]

                def emit_down(r, cc):
                    half, c2 = cc // 2, cc % 2
                    p = r * 2 + half
                    if c2 == 0:
                        load_wd(p)
                        load_wd(p + 1)
                        load_wd(p + 2)
                    wd = wd_piece[p]
                    bk = bankx(2 + dbk[0] % 2)
                    dbk[0] += 1
                    for k in range(16):
                        wk = wd.sub(k * 256 + c2 * 128, k * 256 + (c2 + 1) * 128)
                        mm(bk, bk.ap, wk, wk.ap, hT_k(k), hT_k(k).ap, k == 0, k == 15)
                    dv = gd2[r % GDN].sub(cc * T, (cc + 1) * T)
                    act(dv, dv.ap, bk, bk.ap, AF.Gelu_apprx_tanh)

                cctr = [0]
                chain_slot = {}

                def emit_chainA(r, b):
                    sl = cctr[0] % NCH
                    cctr[0] += 1
                    chain_slot[(r, b)] = sl
                    Sg, Wg = Sg2[sl], Wg2[sl]
                    Eg = Sg
                    scb = sc.sub(b * 2048, (b + 1) * 2048)
                    s4 = scb.ap.rearrange("p (h t k) -> p h t k", h=8, t=2)
                    in0 = s4[:, :, 0, 4 * r:4 * r + 4].unsqueeze(3).to_broadcast([128, 8, 4, 128])
                    in1 = s4[:, :, 1, :].unsqueeze(2).to_broadcast([128, 8, 4, 128])
                    S.op("dve", lambda e, o=Sg.ap.rearrange("p (h i j) -> p h i j", h=8, i=4), a=in0, c=in1:
                         e.tensor_tensor(out=o, in0=a, in1=c, op=ALU.add), reads=[scb], writes=[Sg])
                    act(Eg, Eg.ap, Sg, Sg.ap, AF.Prelu, alpha=1.0e5)
                    act(Wg, Wg.ap, Eg, Eg.ap, AF.Exp)

                def emit_chainB(r, b):
                    pass

                def emit_gt(r, b):
                    Wg = Wg2[chain_slot[(r, b)]]
                    gbk = bankx(b % 2)
                    for cc in range(4):
                        for h in range(8):
                            wv = Wg.sub(h * 512 + cc * 128, h * 512 + (cc + 1) * 128)
                            dv = dgm.sub((b * 8 + h) * 128, (b * 8 + h + 1) * 128)
                            mm(gbk, gbk.ap[:, cc * 128:(cc + 1) * 128], wv, wv.ap, dv, dv.ap, h == 0, h == 7)
                    at = AT2[r % 2]
                    gdv = gd2[r % GDN]
                    oap = at.ap.rearrange("p (c s) -> p c s", s=T)[:, :, b * 128:(b + 1) * 128]
                    gap = gdv.ap.rearrange("p (c s) -> p c s", s=T)[:, :, b * 128:(b + 1) * 128]
                    tt("dve", at.span(b * 128, 3 * T + (b + 1) * 128, oap), oap, gbk, gbk.ap.rearrange("p (c s) -> p c s", s=128),
                       gdv.span(b * 128, 3 * T + (b + 1) * 128, gap), gap, ALU.mult)

                def emit_up(r, q):
                    cg = q
                    p = r * 4 + cg
                    load_wu(p)
                    load_wu(p + 1)
                    load_wu(p + 2)
                    load_wu(p + 3)
                    wu = wu_piece[p]
                    at = AT2[r % 2]
                    for b in range(NB):
                        bk = bankx(4 + ubk[0] % 4)
                        ubk[0] += 1
                        for cc in range(4):
                            av = at.sub(cc * T + b * 128, cc * T + (b + 1) * 128)
                            wv = wu.sub(cc * 512, (cc + 1) * 512)
                            mm(bk, bk.ap, av, av.ap, wv, wv.ap, cc == 0, cc == 3)
                        xv = xs.sub(b * D + cg * 512, b * D + (cg + 1) * 512)
                        tt("dve", xv, xv.ap, bk, bk.ap, xv, xv.ap, ALU.add)

                load_wd(0)
                load_wd(1)
                for r_ in range(GDN - 1):
                    for cc in range(4):
                        emit_down(r_, cc)
                AXX = mybir.AxisListType.X
                for b in range(NB):
                    svs = [sc.sub(b * 2048 + hp * 128, b * 2048 + (hp + 1) * 128) for hp in range(16)]
                    tps = [top.sub((b * 16 + hp) * 16, (b * 16 + hp + 1) * 16) for hp in range(16)]
                    wks = [cand8.sub(hp * 128, (hp + 1) * 128) for hp in range(16)]
                    for hp in range(16):
                        S.op("dve", lambda e, o=tps[hp].ap[:, 0:8], i=svs[hp].ap: e.max(out=o, in_=i), reads=[svs[hp]], writes=[tps[hp].sub(0, 8)])
                    for hp in range(16):
                        S.op("dve", lambda e, o=wks[hp].ap, r=tps[hp].ap[:, 0:8], i=svs[hp].ap: e.match_replace(out=o, in_to_replace=r, in_values=i, imm_value=NEG),
                             reads=[svs[hp], tps[hp].sub(0, 8)], writes=[wks[hp]])
                    for hp in range(16):
                        S.op("dve", lambda e, o=tps[hp].ap[:, 8:16], i=wks[hp].ap: e.max(out=o, in_=i), reads=[wks[hp]], writes=[tps[hp].sub(8, 16)])
                    tb = top.sub(b * 256, (b + 1) * 256)
                    scb_ = sc.sub(b * 2048, (b + 1) * 2048)
                    tt("dve", penb, penb.ap.rearrange("p (q k) -> p q k", k=128), scb_, scb_.ap.rearrange("p (q k) -> p q k", k=128),
                       tb, tb.ap.rearrange("p (q k) -> p q k", k=16)[:, :, 15:16].to_broadcast([128, 16, 128]), ALU.is_lt)
                    stt(scb_, scb_.ap, penb, penb.ap, -1.0e4, scb_, scb_.ap, ALU.mult, ALU.add)
                    t4 = tb.ap.rearrange("p (h t k) -> p h t k", h=8, t=2)
                    tt("pool", cand8, cand8.ap.rearrange("p (h a c) -> p h a c", h=8, a=16), tb, t4[:, :, 0, :].unsqueeze(3).to_broadcast([128, 8, 16, 16]),
                       tb, t4[:, :, 1, :].unsqueeze(2).to_broadcast([128, 8, 16, 16]), ALU.add)
                    cas = [cand8.sub(h * 256, (h + 1) * 256) for h in range(8)]
                    c24s = [c24.sub(h * 24, (h + 1) * 24) for h in range(8)]
                    for rnd in range(3):
                        for h in range(8):
                            S.op("dve", lambda e, o=c24s[h].ap[:, rnd * 8:rnd * 8 + 8], i=cas[h].ap: e.max(out=o, in_=i),
                                 reads=[cas[h]], writes=[c24s[h].sub(rnd * 8, rnd * 8 + 8)])
                        if rnd < 2:
                            for h in range(8):
                                S.op("dve", lambda e, o=cas[h].ap, r=c24s[h].ap[:, rnd * 8:rnd * 8 + 8]: e.match_replace(out=o, in_to_replace=r, in_values=o, imm_value=NEG),
                                     reads=[cas[h], c24s[h].sub(rnd * 8, rnd * 8 + 8)], writes=[cas[h]])
                    c3 = c24.ap.rearrange("p (h k) -> p h k", k=24)
                    thr = pst.sub(b * 8, b * 8 + 8)
                    lz = pst.sub(32 + b * 8, 32 + b * 8 + 8)
                    cbv = pst.sub(64 + b * 8, 64 + b * 8 + 8)
                    tt("dve", thr, thr.ap, c24, c3[:, :, 15], c24, c3[:, :, 16], ALU.add)
                    ts("dve", thr, thr.ap, thr, thr.ap, 0.5, ALU.mult)
                    d16 = candb.sub(0, 128)
                    tt("dve", d16, d16.ap.rearrange("p (h k) -> p h k", k=16), c24, c3[:, :, 0:16], c24, c3[:, :, 0:1].to_broadcast([128, 8, 16]), ALU.subtract)
                    act(d16, d16.ap, d16, d16.ap, AF.Exp)
                    S.op("dve", lambda e, o=lz.ap, i=d16.ap.rearrange("p (h k) -> p h k", k=16): e.tensor_reduce(out=o, in_=i, axis=AXX, op=ALU.add),
                         reads=[d16], writes=[lz])
                    act(lz, lz.ap, lz, lz.ap, AF.Ln)
                    tt("dve", cbv, cbv.ap, thr, thr.ap, c24, c3[:, :, 0], ALU.subtract)
                    tt("dve", cbv, cbv.ap, cbv, cbv.ap, lz, lz.ap, ALU.subtract)
                    ev = ecb.sub(b * 8, b * 8 + 8)
                    act(ev, ev.ap, cbv, cbv.ap, AF.Exp)
                    scb = sc.sub(b * 2048, (b + 1) * 2048)
                    s0ap = scb.ap.rearrange("p (h t k) -> p h t k", h=8, t=2)[:, :, 0, :]
                    tt("dve", scb.span(0, 2048, s0ap), s0ap, scb.span(0, 2048, s0ap), s0ap, thr, thr.ap.unsqueeze(2).to_broadcast([128, 8, 128]), ALU.subtract)
                    dv = dgm.sub(b * 1024, (b + 1) * 1024)
                    tt("pool", dv, dv.ap.rearrange("p (h s) -> p h s", s=128), identb, identb.ap.unsqueeze(1).to_broadcast([128, 8, 128]),
                       ev, ev.ap.unsqueeze(2).to_broadcast([128, 8, 128]), ALU.mult)
                if stop == "topk":
                    dump("sc", sc, [128, NB * 2048])
                    dump("top", top, [128, NB * 256])
                    dump("pst", pst, [128, NB * 32])
                    dump("ecb", ecb, [128, NB * 8])
                    dump("hT", hT, [128, 16 * T], BF16)
                checkpoint("topk")

                load_wu(0)
                load_wu(1)
                def blk(n):
                    return (n // NB, n % NB)

                NBLK = NR * NB
                for n0 in range(NCH):
                    emit_chainA(*blk(n0))
                for n in range(NBLK):
                    r, b = blk(n)
                    emit_gt(r, b)
                    if n + NCH < NBLK:
                        emit_chainA(*blk(n + NCH))
                    if r + GDN - 1 < NR and b % 2 == 0:
                        emit_down(r + GDN - 1, b)
                        emit_down(r + GDN - 1, b + 1)
                    if r >= 1:
                        emit_up(r - 1, b)
                for q in range(4):
                    emit_up(NR - 1, q)

                nfg = View(abuf, Sg2[0].lo, Sg2[0].lo + D * 4, A[:, Sg2[0].lo // 4:Sg2[0].lo // 4 + D], F32)
                dma(nfg, DR(gf_d.partition_broadcast(128)), "nfg")
                for b in range(NB):
                    xb = xs.sub(b * D, (b + 1) * D)
                    ss = stat.sub(16 + b, 17 + b)
                    rs = stat.sub(24 + b, 25 + b)
                    jk = Wg2[0].sub(0, D)
                    memset("pool", ss, ss.ap, 0.0)
                    act(jk, jk.ap, xb, xb.ap, AF.Square, extra_r=[ss], extra_w=[ss], accum_out=ss.ap)
                    ts("dve", rs, rs.ap, ss, ss.ap, 1.0 / D, ALU.mult, EPS, ALU.add)
                    act(rs, rs.ap, rs, rs.ap, AF.Sqrt)
                    S.op("dve", lambda e, o=rs.ap: e.reciprocal(out=o, in_=o), reads=[rs], writes=[rs])
                    osrc = (Sg2[1], Wg2[1])[b % 2]
                    ost = View(abuf, osrc.lo, osrc.lo + D * 4, A[:, osrc.lo // 4:osrc.lo // 4 + D], F32)
                    stt(ost, ost.ap, xb, xb.ap, rs.ap, nfg, nfg.ap, ALU.mult, ALU.mult, extra_r=[rs])
                    dma(View(ybuf, r0 + b * 128, r0 + (b + 1) * 128, y_d[r0 + b * 128:r0 + (b + 1) * 128, :]), ost, "ys%d" % (b % 2))

        except _Stop:
            pass
        S.dma_barrier("sp")
        S.emit(st)
        info = dict(nops=len(S.ops), nwait=S.nwait, nsem=S.nsem, arena=off[0])
    return nc, info


_CACHE = {}


def _prep_shared(inputs):
    f = lambda a: np.ascontiguousarray(np.asarray(a, dtype=np.float32))
    sh = {
        "norm_mix_g": f(inputs["norm_mix_g"]).reshape(D),
        "norm_mem_g": f(inputs["norm_mem_g"]).reshape(D),
        "w_in": f(inputs["w_in"]).reshape(D, DIN),
        "gmlp_w_s": f(inputs["gmlp_w_s"]).reshape(8, 128, 128),
        "gmlp_b_s": f(inputs["gmlp_b_s"]).reshape(8 * 128),
        "gmlp_ln_g": f(inputs["gmlp_ln_g"]).reshape(1024),
        "gmlp_ln_b": f(inputs["gmlp_ln_b"]).reshape(1024),
        "conv_w": f(inputs["conv_w"]).reshape(3, 1024),
        "w_kv": f(inputs["w_kv"]).reshape(D, 2048),
        "w_branch": f(inputs["w_branch"]).reshape(3, 1024, D),
        "w_out": f(inputs["w_out"]).reshape(D, D),
        "norm_ffn_g": f(inputs["norm_ffn_g"]).reshape(D),
        "peer_w_q": f(inputs["peer_w_q"]).reshape(D, 2048),
        "peer_keys": f(inputs["peer_keys"]).reshape(16, 128, 128),
        "peer_w_down": f(inputs["peer_w_down"]).reshape(NEXP, D),
        "peer_w_up": f(inputs["peer_w_up"]).reshape(NEXP, D),
        "norm_final_g": f(inputs["norm_final_g"]).reshape(D),
        "ident": np.eye(128, dtype=np.float32),
    }
    return sh


def kernel(**inputs):
    x = np.asarray(inputs["x"], dtype=np.float32)
    mem = np.asarray(inputs["mem"], dtype=np.float32)
    B, Sq, _ = x.shape
    ncores = 8
    per = (B * Sq) // ncores
    halves = Sq // per
    if "nc" not in _CACHE:
        _CACHE["nc"] = build(ntiles=per // T)[0]
    nc = _CACHE["nc"]
    sh = _prep_shared(inputs)
    in_maps = []
    for c in range(ncores):
        b, hf = c // halves, c % halves
        s0 = hf * per
        xh = np.zeros((2, D), np.float32)
        if s0 > 0:
            xh[:] = x[b, s0 - 2:s0]
        m = dict(sh)
        m["x"] = np.ascontiguousarray(x[b, s0:s0 + per])
        m["xh"] = xh
        m["mem"] = np.ascontiguousarray(mem[b])
        in_maps.append(m)
    res = run_bass_kernel_spmd(nc, in_maps, core_ids=list(range(ncores)))
    out = np.empty((B, Sq, D), np.float32)
    for c in range(ncores):
        b, hf = c // halves, c % halves
        out[b, hf * per:(hf + 1) * per] = res.results[c]["y"]
    return out
```
